# Optimizing a Trainium2 kernel written in Bass

```python
import jax, jax.numpy as jnp
from jax import lax
import numpy as np

D_MODEL = 1024
BATCH = 8
SEQ = 2048
DEPTH = 4

GRID_W = 64
CTX_LEN = 256
A_HEADS = 8
A_KV_HEADS = 2
A_HEAD_DIM = 64
ROPE_THETA = 10000.0
Q_BLOCK = 128
B_HEADS = 4
B_HEAD_DIM = 128
B_CHUNK = 128
B_CONV = 3
POOL_WINDOWS = (2, 4, 8, 16)
POOL_GROUP = D_MODEL // 4
N_EXPERTS = 16
EC_FACTOR = 2
D_EXPERT = 1024
NORM_EPS = 1e-6

A_Q = A_HEADS * A_HEAD_DIM
A_KV = A_KV_HEADS * A_HEAD_DIM
B_W = B_HEADS * B_HEAD_DIM
N_GATES = 2 * 2 * B_HEADS
EVEN_IN = A_Q + 2 * A_KV + 4 * B_W + N_GATES
MIX_W = A_Q + B_W
EVEN_SPLITS = (A_Q, A_Q + A_KV, A_Q + 2 * A_KV, A_Q + 2 * A_KV + B_W, A_Q + 2 * A_KV + 2 * B_W,
               A_Q + 2 * A_KV + 3 * B_W, A_Q + 2 * A_KV + 4 * B_W)
N_EVEN = (DEPTH + 1) // 2
N_ODD = DEPTH // 2

kernel_name = "hybrid_diffusion_gqa_mlstm_pool_ecmoe"


def rmsnorm(x, g):
    xf = x.astype(jnp.float32)
    y = xf * lax.rsqrt(jnp.mean(xf * xf, axis=-1, keepdims=True) + NORM_EPS)
    return (y * g.astype(jnp.float32)).astype(x.dtype)


def adaln(cond, w, b):
    m = jax.nn.silu(cond) @ w + b
    return m.reshape(cond.shape[:-1] + (1, 6, D_MODEL))


def modulate(x, g, shift, scale):
    return rmsnorm(x, g) * (1 + scale) + shift


def axial_rope(n_tokens):
    rows = n_tokens // GRID_W
    row_ids = jnp.repeat(jnp.arange(rows, dtype=jnp.float32), GRID_W)
    col_ids = jnp.tile(jnp.arange(GRID_W, dtype=jnp.float32), rows)
    half = A_HEAD_DIM // 2
    inv = ROPE_THETA ** (-jnp.arange(0, half, 2, dtype=jnp.float32) / half)
    ang = jnp.stack([row_ids[:, None] * inv, col_ids[:, None] * inv], axis=1)
    return jnp.cos(ang), jnp.sin(ang)


def apply_rope(x, cos, sin):
    Bn, n, H, d = x.shape
    xr = x.reshape(Bn, n, H, 2, 2, d // 4)
    x1, x2 = xr[..., 0, :], xr[..., 1, :]
    c = cos[None, :, None].astype(x.dtype)
    s = sin[None, :, None].astype(x.dtype)
    return jnp.stack([x1 * c - x2 * s, x2 * c + x1 * s], axis=-2).reshape(x.shape)


def attend_blocks(q, k, v):
    Bn, Lq, _, d = q.shape
    G = A_HEADS // A_KV_HEADS
    nb = Lq // Q_BLOCK
    qb = q.reshape(Bn, nb, Q_BLOCK, A_KV_HEADS, G, d).transpose(1, 0, 3, 4, 2, 5)
    kt = jnp.swapaxes(k, 1, 2)
    vt = jnp.swapaxes(v, 1, 2)
    scale = d ** -0.5

    def one_block(qblk):
        s = jnp.einsum('bhgtd,bhsd->bhgts', qblk, kt).astype(jnp.float32) * scale
        p = jax.nn.softmax(s, axis=-1).astype(vt.dtype)
        return jnp.einsum('bhgts,bhsd->bhgtd', p, vt)

    o = lax.map(one_block, qb)
    return o.transpose(1, 0, 4, 2, 3, 5).reshape(Bn, Lq, A_HEADS * d)


def short_conv(x, w):
    n = x.shape[1]
    pad = B_CONV // 2
    xp = jnp.pad(x, ((0, 0), (pad, B_CONV - 1 - pad), (0, 0)))
    return sum(xp[:, j:j + n] * w[j] for j in range(B_CONV))


def mlstm_zero_state(Bn):
    return (jnp.zeros((Bn, B_HEADS, B_HEAD_DIM, B_HEAD_DIM), jnp.float32),
            jnp.zeros((Bn, B_HEADS, B_HEAD_DIM), jnp.float32),
            jnp.full((Bn, B_HEADS), -1e30, jnp.float32))


def mlstm_scan(q, k, v, ig, lf, state):
    Bn, H, L, d = q.shape
    nc = L // B_CHUNK
    k = k * (d ** -0.5)

    def to_chunks(a):
        a = a.reshape(a.shape[:2] + (nc, B_CHUNK) + a.shape[3:])
        return jnp.moveaxis(a, 2, 0)

    tril = jnp.tril(jnp.ones((B_CHUNK, B_CHUNK), dtype=bool))

    def step(carry, inp):
        C, n, m = carry
        qc, kc, vc, ic, fc = inp
        b = jnp.cumsum(fc, axis=-1)
        log_d = jnp.where(tril, b[..., :, None] - b[..., None, :] + ic[..., None, :], -jnp.inf)
        log_inter = m[..., None] + b
        m_t = jnp.maximum(log_inter, jnp.max(log_d, axis=-1))
        w_intra = jnp.exp(log_d - m_t[..., None]) * jnp.einsum('bhtd,bhsd->bhts', qc, kc)
        w_inter = jnp.exp(log_inter - m_t)
        num = (w_inter[..., None] * jnp.einsum('bhed,bhtd->bhte', C, qc)
               + jnp.einsum('bhts,bhse->bhte', w_intra, vc))
        den = w_inter * jnp.einsum('bhd,bhtd->bht', n, qc) + jnp.sum(w_intra, axis=-1)
        h = num / jnp.maximum(jnp.abs(den), jnp.exp(-m_t))[..., None]
        b_end = b[..., -1]
        log_w = b_end[..., None] - b + ic
        m_new = jnp.maximum(m + b_end, jnp.max(log_w, axis=-1))
        w_s = jnp.exp(log_w - m_new[..., None])
        decay = jnp.exp(m + b_end - m_new)
        C = decay[..., None, None] * C + jnp.einsum('bhs,bhse,bhsd->bhed', w_s, vc, kc)
        n = decay[..., None] * n + jnp.einsum('bhs,bhsd->bhd', w_s, kc)
        return (C, n, m_new), h

    state, h = lax.scan(step, state, tuple(to_chunks(a) for a in (q, k, v, ig, lf)))
    h = jnp.moveaxis(h, 0, 2).reshape(Bn, H, L, d)
    return h, state


def mlstm_bidir(q, k, v, ig, lf, states):
    h_f, s_f = mlstm_scan(q, k, v, ig[0], lf[0], states[0])
    flip = lambda a: jnp.flip(a, axis=2)
    h_b, s_b = mlstm_scan(flip(q), flip(k), flip(v), flip(ig[1]), flip(lf[1]), states[1])
    return h_f + flip(h_b), (s_f, s_b)


def even_mixer(h_lat, h_ctx, ctx_out, w_in, w_out, g_qn, g_kn, w_conv, b_gate, g_hn):
    Bn, L, _ = h_lat.shape

    def project(h):
        n = h.shape[1]
        qa, ka, va, qb, kb, vb, ob, gt = jnp.split(h @ w_in, EVEN_SPLITS, axis=-1)
        qa = rmsnorm(qa.reshape(Bn, n, A_HEADS, A_HEAD_DIM), g_qn)
        ka = rmsnorm(ka.reshape(Bn, n, A_KV_HEADS, A_HEAD_DIM), g_kn)
        va = va.reshape(Bn, n, A_KV_HEADS, A_HEAD_DIM)
        qk = jax.nn.silu(short_conv(jnp.concatenate([qb, kb], axis=-1), w_conv))
        to_heads = lambda a: a.reshape(Bn, n, B_HEADS, B_HEAD_DIM).transpose(0, 2, 1, 3).astype(jnp.float32)
        qm, km, vm = to_heads(qk[..., :B_W]), to_heads(qk[..., B_W:]), to_heads(vb)
        g = (gt + b_gate).astype(jnp.float32).reshape(Bn, n, 2, 2, B_HEADS).transpose(2, 3, 0, 4, 1)
        ig = g[:, 0]
        lf = jax.nn.log_sigmoid(g[:, 1])
        return (qa, ka, va), (qm, km, vm, ig, lf), ob

    def mlstm_out(hm, ob):
        n = hm.shape[2]
        hm = rmsnorm(jnp.swapaxes(hm, 1, 2), g_hn.reshape(B_HEADS, B_HEAD_DIM))
        return hm.reshape(Bn, n, B_W).astype(ob.dtype) * jax.nn.sigmoid(ob)

    (qa_c, ka_c, va_c), mc, ob_c = project(h_ctx)
    (qa_l, ka_l, va_l), ml, ob_l = project(h_lat)
    cos, sin = axial_rope(L)
    qa_l = apply_rope(qa_l, cos, sin)
    ka_l = apply_rope(ka_l, cos, sin)
    o_a = attend_blocks(qa_l, jnp.concatenate([ka_c, ka_l], axis=1), jnp.concatenate([va_c, va_l], axis=1))
    st0 = mlstm_zero_state(Bn)
    h_c, ctx_states = mlstm_bidir(*mc, (st0, st0))
    h_l, _ = mlstm_bidir(*ml, ctx_states)
    y = jnp.concatenate([o_a, mlstm_out(h_l, ob_l)], axis=-1) @ w_out
    yc = None
    if ctx_out:
        yc = jnp.concatenate([attend_blocks(qa_c, ka_c, va_c), mlstm_out(h_c, ob_c)], axis=-1) @ w_out
    return y, yc


def pool_mixer(h, w_in, w_grp, s_pool):
    Bn, n, _ = h.shape
    u = h @ w_in
    uf = u.astype(jnp.float32)
    S = jnp.pad(jnp.cumsum(uf, axis=1), ((0, 0), (1, 0), (0, 0)))
    t = jnp.arange(n)
    outs = []
    for gi, w in enumerate(POOL_WINDOWS):
        lo = w // 2
        hi = w - 1 - lo
        a = jnp.clip(t - lo, 0, n - 1)
        e = jnp.clip(t + hi, 0, n - 1)
        Sg = S[:, :, gi * POOL_GROUP:(gi + 1) * POOL_GROUP]
        cnt = (e - a + 1).astype(jnp.float32)[None, :, None]
        outs.append((Sg[:, e + 1] - Sg[:, a]) / cnt - uf[:, :, gi * POOL_GROUP:(gi + 1) * POOL_GROUP])
    p = jnp.stack(outs, axis=2).astype(u.dtype)
    y = jnp.einsum('bngc,gce->bnge', p, w_grp).reshape(Bn, n, D_MODEL)
    return y * s_pool


def ec_moe(h, w_router, w_gate, w_up, w_down):
    Bn, n, Dm = h.shape
    cap = max(1, EC_FACTOR * n // N_EXPERTS)
    aff = jax.nn.softmax(jnp.einsum('bnd,de->bne', h, w_router).astype(jnp.float32), axis=-1)
    gate, idx = lax.top_k(jnp.swapaxes(aff, 1, 2), cap)
    xs = jax.vmap(lambda hb, ib: hb[ib])(h, idx)
    a = jax.nn.silu(jnp.einsum('becd,edf->becf', xs, w_gate)) * jnp.einsum('becd,edf->becf', xs, w_up)
    out = jnp.einsum('becf,efd->becd', a, w_down) * gate[..., None].astype(h.dtype)
    return jax.vmap(lambda ob, ib: jnp.zeros((n, Dm), ob.dtype).at[ib.reshape(-1)].add(ob.reshape(-1, Dm)))(out, idx)


def setup_inputs(seed: int = 0) -> dict:
    key = jax.random.key(seed)
    ks = jax.random.split(key, 23)
    nrm = lambda k, shape, s: s * jax.random.normal(k, shape, jnp.float32)
    fb = jnp.linspace(3.0, 6.0, B_HEADS)
    gate_base = jnp.concatenate([jnp.zeros((B_HEADS,)), fb, jnp.zeros((B_HEADS,)), fb])
    return {
        "x": nrm(ks[0], (BATCH, SEQ, D_MODEL), 1.0),
        "c": nrm(ks[1], (BATCH, D_MODEL), 1.0),
        "ctx": nrm(ks[2], (BATCH, CTX_LEN, D_MODEL), 1.0),
        "c_ctx": nrm(ks[3], (D_MODEL,), 1.0),
        "w_mod": nrm(ks[4], (DEPTH, D_MODEL, 6 * D_MODEL), 0.5 * D_MODEL ** -0.5),
        "b_mod": nrm(ks[5], (DEPTH, 6 * D_MODEL), 0.02),
        "g_norm1": 1.0 + nrm(ks[6], (DEPTH, D_MODEL), 0.02),
        "g_norm2": 1.0 + nrm(ks[7], (DEPTH, D_MODEL), 0.02),
        "w_in_even": nrm(ks[8], (N_EVEN, D_MODEL, EVEN_IN), D_MODEL ** -0.5),
        "w_out_even": nrm(ks[9], (N_EVEN, MIX_W, D_MODEL), MIX_W ** -0.5),
        "g_qnorm": 1.0 + nrm(ks[10], (N_EVEN, A_HEAD_DIM), 0.02),
        "g_knorm": 1.0 + nrm(ks[11], (N_EVEN, A_HEAD_DIM), 0.02),
        "w_conv": nrm(ks[12], (N_EVEN, B_CONV, 2 * B_W), B_CONV ** -0.5),
        "b_gate": gate_base[None] + nrm(ks[13], (N_EVEN, N_GATES), 0.1),
        "g_hnorm": 1.0 + nrm(ks[14], (N_EVEN, B_W), 0.02),
        "w_in_odd": nrm(ks[15], (N_ODD, D_MODEL, D_MODEL), D_MODEL ** -0.5),
        "w_pool_grp": nrm(ks[16], (N_ODD, 4, POOL_GROUP, POOL_GROUP), POOL_GROUP ** -0.5),
        "s_pool": 1.0 + nrm(ks[17], (N_ODD, D_MODEL), 0.1),
        "w_router": nrm(ks[18], (DEPTH, D_MODEL, N_EXPERTS), D_MODEL ** -0.5),
        "w_exp_gate": nrm(ks[19], (DEPTH, N_EXPERTS, D_MODEL, D_EXPERT), D_MODEL ** -0.5),
        "w_exp_up": nrm(ks[20], (DEPTH, N_EXPERTS, D_MODEL, D_EXPERT), D_MODEL ** -0.5),
        "w_exp_down": nrm(ks[21], (DEPTH, N_EXPERTS, D_EXPERT, D_MODEL), D_EXPERT ** -0.5),
        "g_final": 1.0 + nrm(ks[22], (D_MODEL,), 0.02),
    }


def reference(x, c, ctx, c_ctx, w_mod, b_mod, g_norm1, g_norm2, w_in_even, w_out_even, g_qnorm, g_knorm,
              w_conv, b_gate, g_hnorm, w_in_odd, w_pool_grp, s_pool, w_router, w_exp_gate, w_exp_up,
              w_exp_down, g_final):
    xc = ctx
    for i in range(DEPTH):
        ctx_next = any(j % 2 == 0 for j in range(i + 1, DEPTH))
        ctx_used = (i % 2 == 0) or ctx_next
        m_lat = adaln(c, w_mod[i], b_mod[i])
        h = modulate(x, g_norm1[i], m_lat[..., 0, :], m_lat[..., 1, :])
        yc = None
        if ctx_used:
            m_ctx = adaln(c_ctx, w_mod[i], b_mod[i])
            hc = modulate(xc, g_norm1[i], m_ctx[..., 0, :], m_ctx[..., 1, :])
        if i % 2 == 0:
            e = i // 2
            y, yc = even_mixer(h, hc, ctx_next, w_in_even[e], w_out_even[e], g_qnorm[e], g_knorm[e],
                               w_conv[e], b_gate[e], g_hnorm[e])
        else:
            o = i // 2
            y = pool_mixer(h, w_in_odd[o], w_pool_grp[o], s_pool[o])
            if ctx_next:
                yc = pool_mixer(hc, w_in_odd[o], w_pool_grp[o], s_pool[o])
        x = x + m_lat[..., 2, :] * y
        h = modulate(x, g_norm2[i], m_lat[..., 3, :], m_lat[..., 4, :])
        x = x + m_lat[..., 5, :] * ec_moe(h, w_router[i], w_exp_gate[i], w_exp_up[i], w_exp_down[i])
        if ctx_next:
            xc = xc + m_ctx[..., 2, :] * yc
            hc = modulate(xc, g_norm2[i], m_ctx[..., 3, :], m_ctx[..., 4, :])
            xc = xc + m_ctx[..., 5, :] * ec_moe(hc, w_router[i], w_exp_gate[i], w_exp_up[i], w_exp_down[i])
    return rmsnorm(x, g_final)
```

```python
from contextlib import ExitStack
import numpy as np
import concourse.bass as bass
import concourse.mybir as mybir
from concourse.bass_utils import run_bass_kernel_spmd

F32 = mybir.dt.float32
BF16 = mybir.dt.bfloat16
AF = mybir.ActivationFunctionType
ALU = mybir.AluOpType

PE, ACT, DVE, POOL, SP = "tensor", "scalar", "vector", "gpsimd", "sync"
ENGS = (PE, ACT, DVE, POOL, SP)
CENGS = (PE, ACT, DVE, POOL)

D = 1024
NT = 2304
TC = 256
TL = 2048
DEPTH = 4
NE = 16
EPS = 1e-6
CTX_NEXT = [True, True, False, False]
CTX_USED = [True, True, True, False]
BLOCKS = [(0, 256), (256, 768), (768, 1280), (1280, 1792), (1792, 2304)]
SEGS = [(0, 256), (256, 2304)]
POOL_W = (2, 4, 8, 16)

VO = {}
_o = 0
for _n, _c in (("bmod", 192), ("gn1", 32), ("gn2", 32), ("gfin", 8), ("spool", 16), ("wconv", 48),
               ("ghn", 8), ("gq", 2), ("gk", 2), ("cvec", 8), ("cctx", 8)):
    VO[_n] = _o
    _o += _c
NV = 384
CB = {"ident": 0, "ones": 128, "bd64": 256, "rperm": 384, "tril": 512, "triu": 640, "iotaf": 768, "sel": 1024}
NCB = 1024 + 2048
CF = {"ident": 0, "tril": 128, "triu": 256, "ones": 384, "iotac": 512, "epsc": 514, "onec": 515, "zeroc": 516}
NCF = 520


class Prog:
    def __init__(self, nc, stack):
        self.nc = nc
        self.stack = stack
        self.ops = {e: [] for e in ENGS}
        self.esem = {e: stack.enter_context(nc.semaphore("es_" + e)) for e in CENGS}
        self.ecount = {e: 0 for e in CENGS}
        self.seen = {e: {} for e in ENGS}
        self.lastw = {}
        self.readers = {}
        self.dsem = {}
        self.semobj = {}
        self.dcum = {}

    def _sem_for_key(self, key):
        if key not in self.dsem:
            s = self.stack.enter_context(self.nc.semaphore("ds%d" % len(self.dsem)))
            name = "ds_" + str(key)
            self.dsem[key] = [s, 0, name]
            self.semobj[name] = s
        return self.dsem[key]

    def _need(self, eng, tok, waits):
        if tok is None:
            return
        name, val = tok
        if name == "es_" + eng and eng == PE:
            return
        if name.startswith("ds_"):
            val = self.dcum[name]
        if self.seen[eng].get(name, 0) >= val:
            return
        self.seen[eng][name] = val
        waits[name] = max(waits.get(name, 0), val)

    def _wl(self, waits):
        wl = []
        for name, val in waits.items():
            sem = self.semobj[name] if name.startswith("ds_") else self.esem[name[3:]]
            wl.append((sem, val))
        return wl

    def op(self, eng, fn, reads=(), writes=(), dma_key=None, inc=True):
        waits = {}
        for k in reads:
            self._need(eng, self.lastw.get(k), waits)
        for k in writes:
            self._need(eng, self.lastw.get(k), waits)
            for t in self.readers.get(k, ()):
                self._need(eng, t, waits)
        if dma_key is not None:
            ent = self._sem_for_key(dma_key)
            ent[1] += 16
            self.dcum[ent[2]] = ent[1]
            tok = (ent[2], ent[1])
            inc = (ent[0], 16)
        elif eng == PE and not inc:
            tok = ("es_" + eng, self.ecount[eng] + 1)
            inc = None
        else:
            self.ecount[eng] += 1
            tok = ("es_" + eng, self.ecount[eng])
            inc = (self.esem[eng], 1)
        self.ops[eng].append((self._wl(waits), fn, inc))
        for k in writes:
            self.lastw[k] = tok
            self.readers[k] = []
        for k in reads:
            self.readers.setdefault(k, []).append(tok)
        return tok

    def barrier(self):
        for e in ENGS:
            waits = {}
            for o in CENGS:
                if self.ecount[o]:
                    self._need(e, ("es_" + o, self.ecount[o]), waits)
            for key, ent in self.dsem.items():
                if ent[1]:
                    self._need(e, (ent[2], ent[1]), waits)
            self.ops[e].append((self._wl(waits), None, None))
        self.lastw = {}
        self.readers = {}

    def emit(self):
        with self.nc.Block() as block:
            def mk(e):
                def body(engine):
                    for wl, fn, inc in self.ops[e]:
                        for sem, val in wl:
                            engine.wait_ge(sem, val)
                        if fn is not None:
                            ins = fn(engine)
                            if inc is not None:
                                ins.then_inc(inc[0], inc[1])
                return body
            block.tensor(mk(PE))
            block.scalar(mk(ACT))
            block.vector(mk(DVE))
            block.gpsimd(mk(POOL))
            block.sync(mk(SP))


class Arena:
    def __init__(self, ap, nwords):
        self.ap = ap
        self.n = nwords
        self.off = 0

    def reset(self, off=0):
        self.off = off

    def alloc(self, shape, dtype):
        nel = int(np.prod(shape))
        words = nel if dtype == F32 else (nel + 1) // 2
        words += words & 1
        a = self.ap[:, self.off:self.off + words]
        self.off += words
        assert self.off <= self.n, ("arena overflow", self.off, self.n)
        if dtype == BF16:
            a = a.bitcast(BF16)
        a = a[:, 0:nel]
        if len(shape) == 2:
            a = a.rearrange("p (a b) -> p a b", a=shape[0])
        elif len(shape) == 3:
            a = a.rearrange("p (a b c) -> p a b c", a=shape[0], b=shape[1])
        return a


class K:
    def __init__(self, nc, st, dr):
        self.nc = nc
        self.P = Prog(nc, st)
        self.dr = dr
        self.uid = 0
        sb = lambda n, s, d: st.enter_context(nc.sbuf_tensor(n, s, d))
        self.XT = sb("XT", [128, 8, NT], F32)
        self.CSTB = sb("CSTB", [128, NCB], BF16)
        self.CSTF = sb("CSTF", [128, NCF], F32)
        self.VEC = sb("VEC", [128, NV], F32)
        self.MODX = sb("MODX", [128, DEPTH, 2, 6, 8], F32)
        self.BG = sb("BG", [128, 32], F32)
        self.AR_WORDS = 30400
        self.ARENA = sb("ARENA", [128, self.AR_WORDS], F32)
        self.ar = Arena(self.ARENA, self.AR_WORDS)
        self.banks = [st.enter_context(nc.psum_tensor("pb%d" % i, [128, 512], F32)) for i in range(8)]
        self.bank_lru = list(range(8))

    def key(self, s):
        self.uid += 1
        return "%s#%d" % (s, self.uid)

    def ps_get(self):
        b = self.bank_lru.pop(0)
        return b

    def ps_put(self, b):
        self.bank_lru.append(b)

    def cb(self, name, w=128, rows=128):
        o = CB[name]
        return self.CSTB[0:rows, o:o + w]

    def cf(self, name, w=128, rows=128):
        o = CF[name]
        return self.CSTF[0:rows, o:o + w]

    def mm(self, out, lhsT, rhs, start, stop, r, w):
        self.P.op(PE, lambda e: e.matmul(out, lhsT=lhsT, rhs=rhs, start=start, stop=stop), reads=r, writes=w, inc=bool(stop))

    def tr(self, out, in_, ident, r, w):
        self.P.op(PE, lambda e: e.transpose(out, in_, ident), reads=r, writes=w)

    def act(self, out, in_, func, r, w, bias=None, scale=None, accum_out=None):
        kw = {}
        if accum_out is not None:
            kw["accum_out"] = accum_out
        if bias is not None:
            kw["bias"] = bias
        if scale is not None:
            kw["scale"] = scale
        self.P.op(ACT, lambda e: e.activation(out, in_, func, **kw), reads=r, writes=w)

    def tt(self, eng, out, in0, in1, op, r, w):
        self.P.op(eng, lambda e: e.tensor_tensor(out, in0, in1, op), reads=r, writes=w)

    def ts(self, eng, out, in0, s1, s2, op0, op1, r, w):
        if op1 is None:
            self.P.op(eng, lambda e: e.tensor_scalar(out, in0, s1, None, op0), reads=r, writes=w)
        else:
            self.P.op(eng, lambda e: e.tensor_scalar(out, in0, s1, s2, op0, op1), reads=r, writes=w)

    def stt(self, out, in0, scalar, in1, op0, op1, r, w):
        self.P.op(DVE, lambda e: e.scalar_tensor_tensor(out, in0, scalar, in1, op0, op1), reads=r, writes=w)

    def cp(self, eng, out, in_, r, w):
        if eng == ACT:
            self.P.op(ACT, lambda e: e.copy(out, in_), reads=r, writes=w)
        else:
            self.P.op(eng, lambda e: e.tensor_copy(out, in_), reads=r, writes=w)

    def dma(self, eng, out, in_, r, w, key):
        self.P.op(eng, lambda e: e.dma_start(out=out, in_=in_), reads=r, writes=w, dma_key=key)

    def wload(self, dst, src, key, nparts=1):
        self.dma(POOL, dst, src, [], [key], key)


def build(dbg=False, stop_after=None):
    nc = bass.Bass("TRN2", target_bir_lowering=False)
    dt = lambda n, s, kind="ExternalInput": nc.dram_tensor(n, s, F32, kind=kind).ap()
    dr = {
        "xin": dt("xin", [NT, D]), "vecs": dt("vecs", [NV, 128]), "cstb": dt("cstb", [128, NCB]),
        "cstf": dt("cstf", [128, NCF]), "rope": dt("rope", [2, 128, TL]), "bgate": dt("bgate", [128, 32]),
        "invcnt": dt("invcnt", [4, 128, NT]),
        "w_mod": dt("w_mod", [DEPTH, D, 6 * D]), "w_in_even": dt("w_in_even", [2, D, 2832]),
        "w_out_even": dt("w_out_even", [2, D, D]), "w_in_odd": dt("w_in_odd", [2, D, D]),
        "w_pool_grp": dt("w_pool_grp", [2, 4, 256, 256]), "w_router": dt("w_router", [DEPTH, D, NE]),
        "w_exp_gate": dt("w_exp_gate", [DEPTH, NE, D, D]), "w_exp_up": dt("w_exp_up", [DEPTH, NE, D, D]),
        "w_exp_down": dt("w_exp_down", [DEPTH, NE, D, D]),
        "out": dt("out", [TL, D], kind="ExternalOutput"),
    }
    if dbg:
        dr["dbg"] = dt("dbg", [8, 128, 8, NT], kind="ExternalOutput")
    with ExitStack() as st:
        k = K(nc, st, dr)
        phase_init(k)
        if stop_after != ("init", 0):
            phase_adaln(k)
        for i in range(DEPTH):
            if stop_after in (("init", 0), ("adaln", 0)):
                if dbg:
                    dump(k, 0)
                break
            if i % 2 == 0:
                phase_even(k, i)
            else:
                phase_odd(k, i)
            if dbg:
                dump(k, 2 * i)
            if stop_after == ("mix", i):
                done = True
                break
            phase_moe(k, i)
            if dbg:
                dump(k, 2 * i + 1)
            if stop_after == ("moe", i):
                done = True
                break
        phase_final(k)
        k.P.emit()
    return nc


def dump(k, slot):
    k.dma(SP, k.dr["dbg"][slot], k.XT[:, :, :], ["XT"], [], "dbg")
    k.P.barrier()


def phase_init(k):
    P, dr, ar = k.P, k.dr, k.ar
    ar.reset()
    for h in range(2):
        k.dma(POOL, k.CSTB[:, h * 1536:(h + 1) * 1536], dr["cstb"][:, h * 1536:(h + 1) * 1536], [], ["CSTB"], "CSTB")
    k.dma(SP, k.CSTF[:, :], dr["cstf"][:, :], [], ["CSTF"], "CSTF")
    k.dma(SP, k.BG[:, :], dr["bgate"][:, :], [], ["BG"], "BG")
    vst = ar.alloc([3, 128], F32)
    k.dma(SP, vst, dr["vecs"].rearrange("(a p) n -> p a n", p=128), [], ["vst"], "vst")
    b = k.ps_get()
    pk = "ps%d" % b
    for a in range(3):
        k.tr(k.banks[b][:, a * 128:(a + 1) * 128], vst[:, a, :], k.cf("ident"), ["vst", "CSTF"], [pk])
    k.cp(DVE, k.VEC[:, :], k.banks[b][:, 0:384], [pk], ["VEC"])
    k.ps_put(b)
    xst = [ar.alloc([1024], F32) for _ in range(2)]
    for t in range(18):
        s = xst[t % 2]
        sk = "xst%d" % (t % 2)
        k.dma(SP if t % 2 == 0 else ACT, s, dr["xin"][t * 128:(t + 1) * 128, :], [], [sk], sk)
        for half in range(2):
            b = k.ps_get()
            pk = "ps%d" % b
            for j in range(4):
                c = half * 4 + j
                k.tr(k.banks[b][:, j * 128:(j + 1) * 128], s[:, c * 128:(c + 1) * 128], k.cf("ident"),
                     [sk, "CSTF"], [pk])
            src = k.banks[b][:, :].rearrange("p (a b) -> p a b", a=4)
            k.cp(DVE if half == 0 else ACT, k.XT[:, half * 4:half * 4 + 4, t * 128:(t + 1) * 128], src, [pk], ["XT"])
            k.ps_put(b)
    P.barrier()


def phase_adaln(k):
    P, dr, ar = k.P, k.dr, k.ar
    ar.reset()
    WM = [ar.alloc([8, 1024], BF16) for _ in range(2)]
    SCf = ar.alloc([8, 2], F32)
    SC = ar.alloc([8, 2], BF16)
    MODR = ar.alloc([DEPTH, 48, 2], F32)
    T1 = ar.alloc([8], F32)
    for w, nm in enumerate(("cvec", "cctx")):
        k.act(SCf[:, :, w], k.VEC[:, VO[nm]:VO[nm] + 8], AF.Silu, ["VEC"], ["SCf"])
    k.cp(DVE, SC, SCf, ["SCf"], ["SC"])
    n = 0
    for i in range(DEPTH):
        b = k.ps_get()
        pk = "ps%d" % b
        for j in range(6):
            wk = "WM%d" % (n % 2)
            Wb = WM[n % 2]
            n += 1
            k.wload(Wb, dr["w_mod"][i][:, j * 1024:(j + 1) * 1024].rearrange("(c p) n -> p c n", p=128), wk)
            for f in range(8):
                col = (j * 8 + f) * 2
                for kk in range(8):
                    k.mm(k.banks[b][:, col:col + 2], Wb[:, kk, f * 128:(f + 1) * 128], SC[:, kk, :],
                         kk == 0, kk == 7, [wk, "SC"], [pk])
        src = k.banks[b][:, 0:96].rearrange("p (a b) -> p a b", b=2)
        for w in range(2):
            k.tt(DVE, MODR[:, i, :, w], src[:, :, w], k.VEC[:, VO["bmod"] + i * 48:VO["bmod"] + (i + 1) * 48],
                 ALU.add, [pk, "VEC"], ["MODR"])
        k.ps_put(b)
        for w in range(2):
            m = lambda j: MODR[:, i, j * 8:(j + 1) * 8, w]
            MX = lambda q: k.MODX[:, i, w, q, :]
            for (q, jscale, gname) in ((0, 1, "gn1"), (3, 4, "gn2")):
                k.ts(DVE, T1, m(jscale), 1.0, None, ALU.add, None, ["MODR"], ["T1"])
                k.tt(DVE, MX(q), T1, k.VEC[:, VO[gname] + i * 8:VO[gname] + (i + 1) * 8], ALU.mult,
                     ["T1", "VEC"], ["MODX"])
            k.cp(DVE, MX(1), m(0), ["MODR"], ["MODX"])
            k.cp(DVE, MX(4), m(3), ["MODR"], ["MODX"])
            k.cp(DVE, MX(5), m(5), ["MODR"], ["MODX"])
            if i % 2 == 0:
                k.cp(DVE, MX(2), m(2), ["MODR"], ["MODX"])
            else:
                o = i // 2
                k.tt(DVE, MX(2), m(2), k.VEC[:, VO["spool"] + o * 8:VO["spool"] + (o + 1) * 8], ALU.mult,
                     ["MODR", "VEC"], ["MODX"])
    P.barrier()


def norm_mod(k, i, which, HT, include_ctx=True):
    ar = k.ar
    SQ = [ar.alloc([8, 512], BF16) for _ in range(2)]
    RS = [ar.alloc([512], F32) for _ in range(2)]
    LN = ar.alloc([512], F32)
    TMP = [ar.alloc([512], F32) for _ in range(2)]
    qg, qs = (0, 1) if which == 0 else (3, 4)
    n = 0
    for bi, (t0, t1) in enumerate(BLOCKS):
        if bi == 0 and not include_ctx:
            continue
        w = 1 if bi == 0 else 0
        tw = t1 - t0
        sq = SQ[bi % 2]
        sqk = "SQ%d" % (bi % 2)
        rs = RS[bi % 2]
        rsk = "RS%d" % (bi % 2)
        k.act(sq[:, :, 0:tw], k.XT[:, :, t0:t1], AF.Square, ["XT"], [sqk])
        b = k.ps_get()
        pk = "ps%d" % b
        for c in range(8):
            k.mm(k.banks[b][:, 0:tw], k.cb("ones"), sq[:, c, 0:tw], c == 0, c == 7, [sqk, "CSTB"], [pk])
        k.act(LN[:, 0:tw], k.banks[b][:, 0:tw], AF.Ln, [pk, "CSTF"], ["LN"], bias=k.cf("epsc", 1), scale=1.0 / D)
        k.ps_put(b)
        k.act(rs[:, 0:tw], LN[:, 0:tw], AF.Exp, ["LN"], [rsk], scale=-0.5)
        for c in range(8):
            tmp = TMP[n % 2]
            tk = "TMPn%d" % (n % 2)
            n += 1
            k.stt(tmp[:, 0:tw], k.XT[:, c, t0:t1], k.MODX[:, i, w, qg, c:c + 1], rs[:, 0:tw], ALU.mult, ALU.mult,
                  ["XT", "MODX", rsk], [tk])
            k.act(HT[:, c, t0:t1], tmp[:, 0:tw], AF.Identity, [tk, "MODX"], ["HT"],
                  bias=k.MODX[:, i, w, qs, c:c + 1], scale=1.0)


def phase_odd(k, i):
    P, dr, ar = k.P, k.dr, k.ar
    o = i // 2
    ar.reset()
    inc_ctx = CTX_NEXT[i]
    HT = ar.alloc([8, NT], BF16)
    mark = ar.off
    norm_mod(k, i, 0, HT, include_ctx=inc_ctx)
    P.barrier()
    ar.reset(mark)
    WIN = ar.alloc([8, 1024], BF16)
    WG = ar.alloc([4, 2, 256], BF16)
    PADL, PADR = 16, 8
    UW = PADL + TC + PADR + PADL + TL + PADR
    segoff = [PADL, PADL + TC + PADR + PADL]
    U = [ar.alloc([UW], F32) for _ in range(2)]
    AB = [ar.alloc([UW], F32) for _ in range(2)]
    IC = ar.alloc([NT], F32)
    PP = [ar.alloc([NT], BF16) for _ in range(2)]
    k.wload(WIN, dr["w_in_odd"][o].rearrange("(c p) n -> p c n", p=128), "WIN")
    for g in range(4):
        k.wload(WG[:, g], dr["w_pool_grp"][o][g].rearrange("(c p) n -> p c n", p=128), "WG")
    for j in range(2):
        k.P.op(POOL, lambda e, j=j: e.memset(U[j][:, :], 0.0), writes=["U%d" % j])
        k.P.op(POOL, lambda e, j=j: e.memset(AB[j][:, :], 0.0), writes=["AB%d" % j])
    blocks = BLOCKS if inc_ctx else BLOCKS[1:]
    segs = [0, 1] if inc_ctx else [1]
    for g in range(4):
        wnd = POOL_W[g]
        lo = wnd // 2
        hi = wnd - 1 - lo
        k.dma(SP, IC, dr["invcnt"][g], [], ["IC"], "IC")
        for j in range(2):
            cc = 2 * g + j
            uk = "U%d" % j
            for (t0, t1) in blocks:
                tw = t1 - t0
                b = k.ps_get()
                pk = "ps%d" % b
                for kk in range(8):
                    k.mm(k.banks[b][:, 0:tw], WIN[:, kk, cc * 128:(cc + 1) * 128], HT[:, kk, t0:t1], kk == 0, kk == 7,
                         ["WIN", "HT"], [pk])
                si = 0 if t0 < TC else 1
                off = segoff[si] + (t0 - SEGS[si][0])
                k.cp(ACT, U[j][:, off:off + tw], k.banks[b][:, 0:tw], [pk], [uk])
                k.ps_put(b)
            src, srck = U[j], uk
            nd = 0
            step = 1
            while step < wnd:
                dst, dstk = AB[nd % 2], "AB%d" % (nd % 2)
                k.tt(POOL, dst[:, PADL:UW], src[:, PADL:UW], src[:, PADL - step:UW - step], ALU.add, [srck], [dstk])
                src, srck = dst, dstk
                nd += 1
                step *= 2
            oth, othk = AB[nd % 2], "AB%d" % (nd % 2)
            for si in segs:
                s0, s1 = SEGS[si]
                n = s1 - s0
                so = segoff[si]
                k.tt(DVE, oth[:, so:so + n], src[:, so + hi:so + hi + n], IC[:, s0:s1], ALU.mult, [srck, "IC"], [othk])
                k.tt(DVE, PP[j][:, s0:s1], oth[:, so:so + n], U[j][:, so:so + n], ALU.subtract, [othk, uk], ["PP%d" % j])
        for jj in range(2):
            ec = 2 * g + jj
            for (t0, t1) in blocks:
                tw = t1 - t0
                w = 1 if t0 < TC else 0
                b = k.ps_get()
                pk = "ps%d" % b
                for kc in range(2):
                    k.mm(k.banks[b][:, 0:tw], WG[:, g, kc, jj * 128:(jj + 1) * 128], PP[kc][:, t0:t1], kc == 0, kc == 1,
                         ["WG", "PP%d" % kc], [pk])
                k.stt(k.XT[:, ec, t0:t1], k.banks[b][:, 0:tw], k.MODX[:, i, w, 2, ec:ec + 1], k.XT[:, ec, t0:t1],
                      ALU.mult, ALU.add, [pk, "MODX", "XT"], ["XT"])
                k.ps_put(b)
    P.barrier()


def phase_moe(k, i):
    P, dr, ar = k.P, k.dr, k.ar
    do_ctx = CTX_NEXT[i]
    ar.reset()
    HTOK = ar.alloc([18, 1024], BF16)
    AFT = ar.alloc([18, 16], F32)
    AFTB = ar.alloc([18, 16], BF16)
    POST = ar.alloc([18, 16], F32)
    POSB = ar.alloc([NT], BF16)
    GSB = ar.alloc([4], F32)
    M8 = ar.alloc([8], F32)
    mark_small = ar.off
    HT = ar.alloc([8, NT], BF16)
    mark_ht = ar.off
    norm_mod(k, i, 1, HT, include_ctx=do_ctx)
    P.barrier()
    ar.reset(mark_ht)
    tiles = list(range(18)) if do_ctx else list(range(2, 18))
    WR = ar.alloc([8, 16], BF16)
    ET = ar.alloc([18, 16], F32)
    Z = ar.alloc([18], F32)
    RZ = ar.alloc([18], F32)
    k.wload(WR, dr["w_router"][i].rearrange("(c p) n -> p c n", p=128), "WR")
    b = k.ps_get()
    pk = "ps%d" % b
    for t in tiles:
        for c in range(8):
            k.mm(k.banks[b][:, t * 16:(t + 1) * 16], HT[:, c, t * 128:(t + 1) * 128], WR[:, c, :], c == 0, c == 7,
                 ["HT", "WR"], [pk])
    t0, t1 = tiles[0], tiles[-1] + 1
    k.act(ET[:, t0:t1, :], k.banks[b][:, t0 * 16:t1 * 16].rearrange("p (a b) -> p a b", b=16), AF.Exp, [pk], ["ET"])
    k.ps_put(b)
    k.P.op(DVE, lambda e: e.tensor_reduce(Z[:, t0:t1], ET[:, t0:t1, :], mybir.AxisListType.X, ALU.add),
           reads=["ET"], writes=["Z"])
    k.P.op(DVE, lambda e: e.reciprocal(RZ[:, t0:t1], Z[:, t0:t1]), reads=["Z"], writes=["RZ"])
    for t in tiles:
        k.ts(DVE, AFT[:, t, :], ET[:, t, :], RZ[:, t:t + 1], None, ALU.mult, None, ["ET", "RZ"], ["AFT"])
    k.cp(DVE, AFTB[:, t0:t1, :], AFT[:, t0:t1, :], ["AFT"], ["AFTB"])
    n = 0
    for t in tiles:
        for half in range(2):
            b = k.ps_get()
            pk = "ps%d" % b
            pv = k.banks[b][:, 0:256].bitcast(BF16)
            for j in range(4):
                c = half * 4 + j
                k.tr(pv[:, j * 128:(j + 1) * 128], HT[:, c, t * 128:(t + 1) * 128], k.cb("ident"), ["HT", "CSTB"], [pk])
            k.cp(DVE if n % 2 == 0 else ACT, HTOK[:, t, half * 512:(half + 1) * 512], pv, [pk], ["HTOK"])
            n += 1
            k.ps_put(b)
    P.barrier()
    ar.reset(mark_small)
    AFF = ar.alloc([NT], F32)
    WORK = ar.alloc([NT], F32)
    b = k.ps_get()
    pk = "ps%d" % b
    nb = 0
    for t in tiles:
        k.tr(k.banks[b][0:16, nb * 128:(nb + 1) * 128], AFT[:, t, :], k.cf("ident"), ["AFT", "CSTF"], [pk])
        nb += 1
        if nb == 4 or t == tiles[-1]:
            ts0 = (t - nb + 1) * 128
            k.cp(ACT, AFF[0:16, ts0:ts0 + nb * 128], k.banks[b][0:16, 0:nb * 128], [pk], ["AFF"])
            k.ps_put(b)
            if t != tiles[-1]:
                b = k.ps_get()
                pk = "ps%d" % b
            nb = 0
    segl = [(TC, NT, 256)] + ([(0, TC, 32)] if do_ctx else [])
    for (s0, s1, cap) in segl:
        wk_, af_, m8_ = WORK[0:16, s0:s1], AFF[0:16, s0:s1], M8[0:16, :]
        zb_ = k.cf("zeroc", 1)[0:16, :].to_broadcast([16, s1 - s0])
        k.cp(DVE, wk_, af_, ["AFF"], ["WORK"])
        rounds = cap // 8
        for r in range(rounds):
            k.P.op(DVE, lambda e, wk_=wk_, m8_=m8_: e.max(m8_, wk_), reads=["WORK"], writes=["M8"])
            if r < rounds - 1:
                k.P.op(DVE, lambda e, wk_=wk_, m8_=m8_: e.match_replace(wk_, m8_, wk_, -1.0),
                       reads=["WORK", "M8"], writes=["WORK"])
        k.ts(DVE, wk_, af_, M8[0:16, 7:8], None, ALU.is_ge, None, ["AFF", "M8"], ["WORK"])
        k.P.op(DVE, lambda e, wk_=wk_, af_=af_, zb_=zb_: e.tensor_tensor_scan(af_, wk_, zb_, 0.0, ALU.add, ALU.add),
               reads=["WORK", "CSTF"], writes=["AFF"])
        k.tt(DVE, af_, af_, wk_, ALU.mult, ["AFF", "WORK"], ["AFF"])
        k.ts(DVE, af_, af_, -1.0, None, ALU.add, None, ["AFF"], ["AFF"])
        k.cp(DVE, POSB[0:16, s0:s1], af_, ["AFF"], ["POSB"])
    b = k.ps_get()
    pk = "ps%d" % b
    for t in tiles:
        k.tr(k.banks[b][:, t * 16:(t + 1) * 16], AFF[0:16, t * 128:(t + 1) * 128], k.cf("ident", 16, 16),
             ["AFF", "CSTF"], [pk])
    k.cp(DVE, POST[:, t0:t1, :], k.banks[b][:, t0 * 16:t1 * 16].rearrange("p (a b) -> p a b", b=16), [pk], ["POST"])
    k.ps_put(b)
    P.barrier()
    ar.reset(mark_small)
    NS = 288 if do_ctx else 256
    PEH = ar.alloc([16, 256], BF16)
    PEC = ar.alloc([2, 32], BF16)
    XS = ar.alloc([8, 288], BF16)
    A = ar.alloc([8, 288], BF16)
    SGT = [ar.alloc([288], F32) for _ in range(2)]
    YSB = [ar.alloc([2, 1024], BF16) for _ in range(2)]
    YSC = [ar.alloc([1024], BF16) for _ in range(2)]
    PT = [ar.alloc([2, TL], BF16) for _ in range(2)]
    PTC = [ar.alloc([TC], BF16) for _ in range(2)]
    NSLOT = 5
    WS = [ar.alloc([2048], BF16) for _ in range(NSLOT)]
    wn = [0]

    def wq(src_ap, shape_a):
        s = wn[0] % NSLOT
        wn[0] += 1
        v = WS[s].rearrange("p (a b) -> p a b", a=shape_a)
        k.wload(v, src_ap, "WS%d" % s)
        return v, "WS%d" % s

    for e in range(NE):
        slot = e % 2
        for t in range(16):
            k.ts(DVE if t % 2 == 0 else POOL, PEH[:, t, :], k.cb("iotaf", 256), POST[:, 2 + t, e:e + 1], None,
                 ALU.is_equal, None, ["CSTB", "POST"], ["PEH%d" % t])
        if do_ctx:
            for t in range(2):
                k.ts(DVE, PEC[:, t, :], k.cb("iotaf", 32), POST[:, t, e:e + 1], None, ALU.is_equal, None,
                     ["CSTB", "POST"], ["PEC"])
        b = k.ps_get()
        pk = "ps%d" % b
        for ct in range(2):
            for t in range(16):
                k.mm(k.banks[b][:, ct:ct + 1], PEH[:, t, ct * 128:(ct + 1) * 128], AFTB[:, 2 + t, e:e + 1], t == 0, t == 15,
                     ["PEH%d" % t, "AFTB"], [pk])
        if do_ctx:
            for t in range(2):
                k.mm(k.banks[b][0:32, 2:3], PEC[:, t, :], AFTB[:, t, e:e + 1], t == 0, t == 1, ["PEC", "AFTB"], [pk])
        k.cp(DVE, GSB[:, 0:3], k.banks[b][:, 0:3], [pk], ["GSB"])
        k.ps_put(b)
        for dc in range(8):
            b = k.ps_get()
            pk = "ps%d" % b
            for t in range(16):
                k.mm(k.banks[b][:, 0:256], HTOK[:, 2 + t, dc * 128:(dc + 1) * 128], PEH[:, t, :], t == 0, t == 15,
                     ["HTOK", "PEH%d" % t], [pk])
            if do_ctx:
                for t in range(2):
                    k.mm(k.banks[b][:, 256:288], HTOK[:, t, dc * 128:(dc + 1) * 128], PEC[:, t, :], t == 0, t == 1,
                         ["HTOK", "PEC"], [pk])
            k.cp(ACT if dc % 2 == 0 else DVE, XS[:, dc, 0:NS], k.banks[b][:, 0:NS], [pk], ["XS"])
            k.ps_put(b)
        for q in range(4):
            wg, wgk = wq(dr["w_exp_gate"][i, e][:, q * 256:(q + 1) * 256].rearrange("(c p) n -> p c n", p=128), 8)
            wu, wuk = wq(dr["w_exp_up"][i, e][:, q * 256:(q + 1) * 256].rearrange("(c p) n -> p c n", p=128), 8)
            for ff in range(2):
                f = q * 2 + ff
                bg = k.ps_get()
                bu = k.ps_get()
                for kk in range(8):
                    k.mm(k.banks[bg][:, 0:NS], wg[:, kk, ff * 128:(ff + 1) * 128], XS[:, kk, 0:NS], kk == 0, kk == 7,
                         [wgk, "XS"], ["ps%d" % bg])
                for kk in range(8):
                    k.mm(k.banks[bu][:, 0:NS], wu[:, kk, ff * 128:(ff + 1) * 128], XS[:, kk, 0:NS], kk == 0, kk == 7,
                         [wuk, "XS"], ["ps%d" % bu])
                sg = SGT[f % 2]
                sgk = "SGT%d" % (f % 2)
                k.act(sg[:, 0:NS], k.banks[bg][:, 0:NS], AF.Silu, ["ps%d" % bg], [sgk])
                k.tt(DVE, A[:, f, 0:NS], sg[:, 0:NS], k.banks[bu][:, 0:NS], ALU.mult, [sgk, "ps%d" % bu], ["A"])
                k.ps_put(bg)
                k.ps_put(bu)
        ybanks = [k.ps_get() for _ in range(4)]
        cbanks = [k.ps_get() for _ in range(2)] if do_ctx else []
        for q in range(4):
            wd, wdk = wq(dr["w_exp_down"][i, e][q * 256:(q + 1) * 256, :].rearrange("(c p) n -> p c n", p=128), 2)
            for ff in range(2):
                f = q * 2 + ff
                for ct in range(2):
                    for half in range(2):
                        bb = ybanks[ct * 2 + half]
                        k.mm(k.banks[bb][:, :], A[:, f, ct * 128:(ct + 1) * 128], wd[:, ff, half * 512:(half + 1) * 512],
                             f == 0, f == 7, ["A", wdk], ["ps%d" % bb])
                if do_ctx:
                    for half in range(2):
                        bb = cbanks[half]
                        k.mm(k.banks[bb][0:32, :], A[:, f, 256:288], wd[:, ff, half * 512:(half + 1) * 512],
                             f == 0, f == 7, ["A", wdk], ["ps%d" % bb])
        for ct in range(2):
            for half in range(2):
                bb = ybanks[ct * 2 + half]
                k.act(YSB[slot][:, ct, half * 512:(half + 1) * 512], k.banks[bb][:, :], AF.Identity, ["ps%d" % bb, "GSB"],
                      ["YSB%d" % slot], scale=GSB[:, ct:ct + 1])
                k.ps_put(bb)
        if do_ctx:
            for half in range(2):
                bb = cbanks[half]
                k.act(YSC[slot][0:32, half * 512:(half + 1) * 512], k.banks[bb][0:32, :], AF.Identity, ["ps%d" % bb, "GSB"],
                      ["YSC%d" % slot], scale=GSB[0:32, 2:3])
                k.ps_put(bb)
        sel = k.CSTB[0:16, CB["sel"] + e * 128:CB["sel"] + (e + 1) * 128]
        for tb in range(4):
            b = k.ps_get()
            pk = "ps%d" % b
            k.mm(k.banks[b][:, :], sel, POSB[0:16, TC + tb * 512:TC + (tb + 1) * 512], True, True, ["CSTB", "POSB"], [pk])
            for ct in range(2):
                k.ts(DVE, PT[slot][:, ct, tb * 512:(tb + 1) * 512], k.banks[b][:, :], k.cf("iotac", 2)[:, ct:ct + 1], None,
                     ALU.is_equal, None, [pk, "CSTF"], ["PT%d" % slot])
            k.ps_put(b)
        if do_ctx:
            b = k.ps_get()
            pk = "ps%d" % b
            k.mm(k.banks[b][:, 0:TC], sel, POSB[0:16, 0:TC], True, True, ["CSTB", "POSB"], [pk])
            k.ts(DVE, PTC[slot][0:32, :], k.banks[b][0:32, 0:TC], k.cf("iotac", 2)[0:32, 0:1], None, ALU.is_equal, None,
                 [pk, "CSTF"], ["PTC%d" % slot])
            k.ps_put(b)
        if slot == 1:
            for dmc in range(8):
                for tb in range(4):
                    b = k.ps_get()
                    pk = "ps%d" % b
                    nmm = 0
                    for s in range(2):
                        for ct in range(2):
                            k.mm(k.banks[b][:, :], YSB[s][:, ct, dmc * 128:(dmc + 1) * 128],
                                 PT[s][:, ct, tb * 512:(tb + 1) * 512], nmm == 0, nmm == 3, ["YSB%d" % s, "PT%d" % s], [pk])
                            nmm += 1
                    xs = k.XT[:, dmc, TC + tb * 512:TC + (tb + 1) * 512]
                    k.stt(xs, k.banks[b][:, :], k.MODX[:, i, 0, 5, dmc:dmc + 1], xs, ALU.mult, ALU.add,
                          [pk, "MODX", "XT"], ["XT"])
                    k.ps_put(b)
                if do_ctx:
                    b = k.ps_get()
                    pk = "ps%d" % b
                    for s in range(2):
                        k.mm(k.banks[b][:, 0:TC], YSC[s][0:32, dmc * 128:(dmc + 1) * 128], PTC[s][0:32, :], s == 0, s == 1,
                             ["YSC%d" % s, "PTC%d" % s], [pk])
                    xs = k.XT[:, dmc, 0:TC]
                    k.stt(xs, k.banks[b][:, 0:TC], k.MODX[:, i, 1, 5, dmc:dmc + 1], xs, ALU.mult, ALU.add,
                          [pk, "MODX", "XT"], ["XT"])
                    k.ps_put(b)
    P.barrier()


def phase_final(k):
    P, dr, ar = k.P, k.dr, k.ar
    ar.reset()
    SQ = [ar.alloc([8, 512], BF16) for _ in range(2)]
    RS = [ar.alloc([512], F32) for _ in range(2)]
    LN = ar.alloc([512], F32)
    YT = [ar.alloc([8, 512], F32) for _ in range(2)]
    OST = [ar.alloc([1024], F32) for _ in range(2)]
    n = 0
    for bi, (t0, t1) in enumerate(BLOCKS[1:]):
        sq, sqk = SQ[bi % 2], "SQ%d" % (bi % 2)
        rs, rsk = RS[bi % 2], "RS%d" % (bi % 2)
        yt, ytk = YT[bi % 2], "YT%d" % (bi % 2)
        k.act(sq, k.XT[:, :, t0:t1], AF.Square, ["XT"], [sqk])
        b = k.ps_get()
        pk = "ps%d" % b
        for c in range(8):
            k.mm(k.banks[b][:, :], k.cb("ones"), sq[:, c, :], c == 0, c == 7, [sqk, "CSTB"], [pk])
        k.act(LN, k.banks[b][:, :], AF.Ln, [pk, "CSTF"], ["LN"], bias=k.cf("epsc", 1), scale=1.0 / D)
        k.ps_put(b)
        k.act(rs, LN, AF.Exp, ["LN"], [rsk], scale=-0.5)
        for c in range(8):
            k.stt(yt[:, c, :], k.XT[:, c, t0:t1], k.VEC[:, VO["gfin"] + c:VO["gfin"] + c + 1], rs, ALU.mult, ALU.mult,
                  ["XT", "VEC", rsk], [ytk])
        for tt_ in range(4):
            tok0 = (t0 - TC) + tt_ * 128
            ost, ostk = OST[n % 2], "OST%d" % (n % 2)
            n += 1
            for half in range(2):
                b = k.ps_get()
                pk = "ps%d" % b
                for j in range(4):
                    c = half * 4 + j
                    k.tr(k.banks[b][:, j * 128:(j + 1) * 128], yt[:, c, tt_ * 128:(tt_ + 1) * 128], k.cf("ident"),
                         [ytk, "CSTF"], [pk])
                k.cp(DVE if half == 0 else ACT, ost[:, half * 512:(half + 1) * 512], k.banks[b][:, :], [pk], [ostk])
                k.ps_put(b)
            k.dma(SP, dr["out"][tok0:tok0 + 128, :], ost, [ostk], [], "outd%d" % ((n - 1) % 2))
    P.barrier()


def _consts():
    cstb = np.zeros((128, NCB), np.float32)
    cstb[:, CB["ident"]:CB["ident"] + 128] = np.eye(128)
    cstb[:, CB["ones"]:CB["ones"] + 128] = 1.0
    bd = np.zeros((128, 128), np.float32)
    bd[:64, :64] = 1.0
    bd[64:, 64:] = 1.0
    cstb[:, CB["bd64"]:CB["bd64"] + 128] = bd
    rp = np.zeros((128, 128), np.float32)
    for r in range(128):
        p = (r % 32) // 16
        rp[r, r + 16 if p == 0 else r - 16] = 1.0
    cstb[:, CB["rperm"]:CB["rperm"] + 128] = rp
    s = np.arange(128)
    cstb[:, CB["tril"]:CB["tril"] + 128] = (s[:, None] <= s[None, :])
    cstb[:, CB["triu"]:CB["triu"] + 128] = (s[:, None] >= s[None, :])
    cstb[:, CB["iotaf"]:CB["iotaf"] + 256] = np.arange(256)[None, :]
    for e in range(16):
        cstb[e, CB["sel"] + e * 128:CB["sel"] + (e + 1) * 128] = 1.0
    cstf = np.zeros((128, NCF), np.float32)
    cstf[:, CF["ident"]:CF["ident"] + 128] = np.eye(128)
    cstf[:, CF["tril"]:CF["tril"] + 128] = (s[:, None] <= s[None, :])
    cstf[:, CF["triu"]:CF["triu"] + 128] = (s[:, None] >= s[None, :])
    cstf[:, CF["ones"]:CF["ones"] + 128] = 1.0
    cstf[:, CF["iotac"]] = np.arange(128)
    cstf[:, CF["iotac"] + 1] = np.arange(128) + 128
    cstf[:, CF["epsc"]] = EPS
    cstf[:, CF["onec"]] = 1.0
    rows = TL // 64
    row_ids = np.repeat(np.arange(rows, dtype=np.float32), 64)
    col_ids = np.tile(np.arange(64, dtype=np.float32), rows)
    half = 32
    inv = (np.float32(10000.0) ** (-np.arange(0, half, 2, dtype=np.float32) / np.float32(half))).astype(np.float32)
    rope = np.zeros((2, 128, TL), np.float32)
    for r in range(128):
        i = r % 64
        a, p, j = i // 32, (i % 32) // 16, i % 16
        ang = (row_ids if a == 0 else col_ids) * inv[j]
        rope[0, r] = np.cos(ang.astype(np.float32))
        rope[1, r] = np.sin(ang.astype(np.float32)) * (-1.0 if p == 0 else 1.0)
    invcnt = np.zeros((4, 128, NT), np.float32)
    for g, w in enumerate(POOL_W):
        lo = w // 2
        hi = w - 1 - lo
        for (s0, s1) in SEGS:
            n = s1 - s0
            t = np.arange(n)
            a = np.clip(t - lo, 0, n - 1)
            e = np.clip(t + hi, 0, n - 1)
            invcnt[g, :, s0:s1] = (1.0 / (e - a + 1).astype(np.float32))[None, :]
    return cstb, cstf, rope, invcnt


_CACHE = {}


def kernel(x, c, ctx, c_ctx, w_mod, b_mod, g_norm1, g_norm2, w_in_even, w_out_even, g_qnorm, g_knorm,
           w_conv, b_gate, g_hnorm, w_in_odd, w_pool_grp, s_pool, w_router, w_exp_gate, w_exp_up,
           w_exp_down, g_final, _dbg=False, _stop=None, _ncores=8):
    f = lambda a: np.ascontiguousarray(np.asarray(a, dtype=np.float32))
    x, c, ctx, c_ctx = f(x), f(c), f(ctx), f(c_ctx)
    cstb, cstf, rope, invcnt = _consts()
    vec_common = np.zeros((NV, 128), np.float32)

    def put(name, arr):
        a = f(arr).reshape(-1, 128)
        vec_common[VO[name]:VO[name] + a.shape[0]] = a
    put("bmod", b_mod)
    put("gn1", g_norm1)
    put("gn2", g_norm2)
    put("gfin", g_final)
    put("spool", s_pool)
    put("wconv", w_conv)
    put("ghn", g_hnorm)
    put("gq", np.tile(f(g_qnorm), (1, 2)))
    put("gk", np.tile(f(g_knorm), (1, 2)))
    put("cctx", c_ctx)
    bgate = np.ascontiguousarray(np.tile(f(b_gate).reshape(1, 32), (128, 1)))
    shared = {"cstb": cstb, "cstf": cstf, "rope": rope, "bgate": bgate, "invcnt": invcnt,
              "w_mod": f(w_mod), "w_in_even": f(w_in_even), "w_out_even": f(w_out_even), "w_in_odd": f(w_in_odd),
              "w_pool_grp": f(w_pool_grp), "w_router": f(w_router), "w_exp_gate": f(w_exp_gate),
              "w_exp_up": f(w_exp_up), "w_exp_down": f(w_exp_down)}
    in_maps = []
    for b in range(_ncores):
        v = vec_common.copy()
        v[VO["cvec"]:VO["cvec"] + 8] = c[b].reshape(8, 128)
        m = dict(shared)
        m["xin"] = np.ascontiguousarray(np.concatenate([ctx[b], x[b]], axis=0))
        m["vecs"] = v
        in_maps.append(m)
    key = (_dbg, _stop)
    if key not in _CACHE:
        _CACHE[key] = build(dbg=_dbg, stop_after=_stop)
    nc = _CACHE[key]
    res = run_bass_kernel_spmd(nc, in_maps, core_ids=list(range(_ncores)))
    out = np.stack([np.asarray(r["out"], dtype=np.float32) for r in res.results], axis=0)
    if _dbg:
        return out, [np.asarray(r["dbg"]) for r in res.results]
    return out


def _proj_fm(k, Wb, wk, HT, t0, t1):
    b = k.ps_get()
    for kk in range(8):
        k.mm(k.banks[b][:, 0:t1 - t0], Wb[:, kk, :], HT[:, kk, t0:t1], kk == 0, kk == 7, [wk, "HT"], ["ps%d" % b])
    return b


def phase_even(k, i):
    P, dr, ar = k.P, k.dr, k.ar
    ev = i // 2
    ctx_out = CTX_NEXT[i]
    W_in = dr["w_in_even"][ev]
    ar.reset()
    HT = ar.alloc([8, NT], BF16)
    mark_ht = ar.off
    norm_mod(k, i, 0, HT, include_ctx=True)
    P.barrier()
    ar.reset(mark_ht)
    wcol = lambda c0, c1: W_in[:, c0:c1].rearrange("(c p) n -> p c n", p=128)
    oblocks = BLOCKS if ctx_out else BLOCKS[1:]

    def out_proj(CATx, half):
        WO = ar.alloc([4, 1024], BF16)
        k.wload(WO, dr["w_out_even"][ev][half * 512:(half + 1) * 512, :].rearrange("(c p) n -> p c n", p=128), "WO")
        for dmc in range(8):
            for (t0, t1) in oblocks:
                tw = t1 - t0
                w = 1 if t0 < TC else 0
                b = k.ps_get()
                pk = "ps%d" % b
                for f in range(4):
                    k.mm(k.banks[b][:, 0:tw], WO[:, f, dmc * 128:(dmc + 1) * 128], CATx[:, f, t0:t1], f == 0, f == 3,
                         ["WO", "CAT"], [pk])
                xs = k.XT[:, dmc, t0:t1]
                k.stt(xs, k.banks[b][:, 0:tw], k.MODX[:, i, w, 2, dmc:dmc + 1], xs, ALU.mult, ALU.add,
                      [pk, "MODX", "XT"], ["XT"])
                k.ps_put(b)

    CAT = ar.alloc([4, NT], BF16)
    KT = [ar.alloc([NT], BF16) for _ in range(2)]
    VA = ar.alloc([18, 2, 129], BF16)
    QTb = ar.alloc([4, 512], BF16)
    COS = ar.alloc([512], F32)
    SIN = ar.alloc([512], F32)
    WB = [ar.alloc([8, 128], BF16) for _ in range(3)]
    SQ = ar.alloc([512], BF16)
    LNs = ar.alloc([512], F32)
    RS = ar.alloc([512], F32)
    QN = ar.alloc([512], F32)
    QNB = ar.alloc([512], BF16)
    T1 = ar.alloc([512], F32)
    T2 = ar.alloc([512], F32)
    PTT = [ar.alloc([512], BF16) for _ in range(3)]
    DROW = ar.alloc([512], F32)
    LND = ar.alloc([512], F32)
    RD = ar.alloc([512], F32)
    wbn = [0]

    def wb_load(parts):
        s = wbn[0] % 3
        wbn[0] += 1
        o = 0
        for (c0, c1) in parts:
            k.wload(WB[s][:, :, o:o + (c1 - c0)], wcol(c0, c1), "WB%d" % s)
            o += c1 - c0
        return WB[s], "WB%d" % s

    def qk_norm_rope(b, tw, t0, gname, dest, rope_ok):
        pk = "ps%d" % b
        k.act(SQ[:, 0:tw], k.banks[b][:, 0:tw], AF.Square, [pk], ["SQ"])
        b2 = k.ps_get()
        k.mm(k.banks[b2][:, 0:tw], k.cb("bd64"), SQ[:, 0:tw], True, True, ["SQ", "CSTB"], ["ps%d" % b2])
        k.act(LNs[:, 0:tw], k.banks[b2][:, 0:tw], AF.Ln, ["ps%d" % b2, "CSTF"], ["LNs"], bias=k.cf("epsc", 1),
              scale=1.0 / 64)
        k.ps_put(b2)
        k.act(RS[:, 0:tw], LNs[:, 0:tw], AF.Exp, ["LNs"], ["RS"], scale=-0.5)
        gcol = k.VEC[:, VO[gname] + ev:VO[gname] + ev + 1]
        if not rope_ok:
            k.stt(dest, k.banks[b][:, 0:tw], gcol, RS[:, 0:tw], ALU.mult, ALU.mult, [pk, "VEC", "RS"], ["QK"])
            return
        k.stt(QN[:, 0:tw], k.banks[b][:, 0:tw], gcol, RS[:, 0:tw], ALU.mult, ALU.mult, [pk, "VEC", "RS"], ["QN"])
        k.cp(ACT, QNB[:, 0:tw], QN[:, 0:tw], ["QN"], ["QNB"])
        b3 = k.ps_get()
        k.mm(k.banks[b3][:, 0:tw], k.cb("rperm"), QNB[:, 0:tw], True, True, ["QNB", "CSTB"], ["ps%d" % b3])
        k.tt(POOL, T1[:, 0:tw], QN[:, 0:tw], COS[:, 0:tw], ALU.mult, ["QN", "COS"], ["T1"])
        k.tt(DVE, T2[:, 0:tw], k.banks[b3][:, 0:tw], SIN[:, 0:tw], ALU.mult, ["ps%d" % b3, "SIN"], ["T2"])
        k.ps_put(b3)
        k.tt(DVE, dest, T1[:, 0:tw], T2[:, 0:tw], ALU.add, ["T1", "T2"], ["QK"])

    def load_rope(t0):
        k.dma(SP, COS, dr["rope"][0][:, t0 - TC:t0 - TC + 512], [], ["COS"], "COS")
        k.dma(SP, SIN, dr["rope"][1][:, t0 - TC:t0 - TC + 512], [], ["SIN"], "SIN")

    k.P.op(POOL, lambda e: e.memset(VA[:, :, :, :], 0.0), writes=["VA"])
    k.P.op(POOL, lambda e: e.memset(VA[:, :, :, 0:1], 1.0), writes=["VA"])
    k.P.op(POOL, lambda e: e.memset(VA[:, :, :, 128:129], 1.0), writes=["VA"])
    Wv, wvk = wb_load([(640, 768)])
    for t in range(18):
        b = k.ps_get()
        for kk in range(8):
            k.mm(k.banks[b][:, 0:128], HT[:, kk, t * 128:(t + 1) * 128], Wv[:, kk, :], kk == 0, kk == 7, ["HT", wvk],
                 ["ps%d" % b])
        k.cp(ACT if t % 2 else DVE, VA[:, t, :, 64:128], k.banks[b][:, 0:128].rearrange("p (a b) -> p a b", a=2),
             ["ps%d" % b], ["VA"])
        k.ps_put(b)
    for v, parts in enumerate(([(512, 640)], [(576, 640), (512, 576)])):
        Wk, wkk = wb_load(parts)
        for (t0, t1) in BLOCKS:
            if t0 >= TC:
                load_rope(t0)
            b = _proj_fm(k, Wk, wkk, HT, t0, t1)
            qk_norm_rope(b, t1 - t0, t0, "gk", KT[v][:, t0:t1], t0 >= TC)
            k.ps_put(b)
    for (t0, t1) in oblocks:
        tw = t1 - t0
        is_lat = t0 >= TC
        if is_lat:
            load_rope(t0)
        for qc in range(4):
            Wq, wqk = wb_load([(qc * 128, (qc + 1) * 128)])
            b = _proj_fm(k, Wq, wqk, HT, t0, t1)
            qk_norm_rope(b, tw, t0, "gq", QTb[:, qc, 0:tw], is_lat)
            k.ps_put(b)
        stiles = list(range(18)) if is_lat else [0, 1]
        for h in range(8):
            kv, hf, qc = h // 4, h % 2, h // 2
            KTh = KT[0] if kv == hf else KT[1]
            r0 = hf * 64
            bo = k.ps_get()
            pko = "ps%d" % bo
            M = 65 if hf == 0 else 128
            nst = len(stiles)
            bsl = {}

            def issue_S(si):
                s = stiles[si]
                bs = k.ps_get()
                k.mm(k.banks[bs][:, 0:tw], KTh[r0:r0 + 64, s * 128:(s + 1) * 128], QTb[r0:r0 + 64, qc, 0:tw], True, True,
                     ["QK"], ["ps%d" % bs])
                bsl[si] = bs
            for si in range(min(2, nst)):
                issue_S(si)
            for si, s in enumerate(stiles):
                bs = bsl.pop(si)
                pt = PTT[si % 3]
                ptk = "PTT%d" % (si % 3)
                k.act(pt[:, 0:tw], k.banks[bs][:, 0:tw], AF.Exp, ["ps%d" % bs], [ptk], scale=0.125)
                k.ps_put(bs)
                if si + 2 < nst:
                    issue_S(si + 2)
                lv = VA[:, s, kv, 64:129] if hf == 0 else VA[:, s, kv, 0:128]
                k.mm(k.banks[bo][0:M, 0:tw], lv, pt[:, 0:tw], si == 0, si == nst - 1, ["VA", ptk], [pko])
            drow = 64 if hf == 0 else 0
            k.cp(ACT, DROW[drow:drow + 1, 0:tw], k.banks[bo][drow:drow + 1, 0:tw], [pko], ["DROW"])
            bd = k.ps_get()
            nrow = 64 if hf == 0 else 128
            k.mm(k.banks[bd][0:nrow, 0:tw], k.CSTF[drow:drow + 1, CF["ones"]:CF["ones"] + nrow], DROW[drow:drow + 1, 0:tw],
                 True, True, ["DROW", "CSTF"], ["ps%d" % bd])
            k.act(LND[r0:r0 + 64, 0:tw], k.banks[bd][r0:r0 + 64, 0:tw], AF.Ln, ["ps%d" % bd], ["LND"])
            k.ps_put(bd)
            k.act(RD[r0:r0 + 64, 0:tw], LND[r0:r0 + 64, 0:tw], AF.Exp, ["LND"], ["RD"], scale=-1.0)
            k.tt(DVE, CAT[r0:r0 + 64, qc, t0:t1], k.banks[bo][r0:r0 + 64, 0:tw], RD[r0:r0 + 64, 0:tw], ALU.mult,
                 [pko, "RD"], ["CAT"])
            k.ps_put(bo)
    out_proj(CAT, 0)
    P.barrier()
    ar.reset(mark_ht)

    CAT = ar.alloc([4, NT], BF16)
    mark_cat = ar.off
    GT = ar.alloc([18, 16], F32)
    NLF = ar.alloc([18, 8], F32)
    IG = ar.alloc([18, 8], F32)
    NB = ar.alloc([18, 8], F32)
    NTOT = ar.alloc([18, 8], F32)
    EXA = ar.alloc([18, 8], F32)
    US = ar.alloc([18, 8], F32)
    WSC = ar.alloc([18, 8], F32)
    DEC = ar.alloc([18, 8], F32)
    WB = [ar.alloc([8, 128], BF16) for _ in range(3)]
    VB = ar.alloc([18, 129], BF16)
    PADQ = 2
    XQW = PADQ + TC + PADQ + PADQ + TL + PADQ
    qoff = [PADQ, PADQ + TC + PADQ + PADQ]
    XQ = ar.alloc([XQW], F32)
    ACC = ar.alloc([NT], F32)
    QM = ar.alloc([NT], BF16)
    KM = ar.alloc([NT], BF16)
    KW = [[ar.alloc([128], BF16) for _ in range(2)] for _ in range(2)]
    HDb = ar.alloc([NT], BF16)
    HM = XQ
    CT = [ar.alloc([129], F32) for _ in range(2)]
    CTB = [[ar.alloc([128], BF16) for _ in range(2)] for _ in range(2)]
    NREP = [[ar.alloc([128], BF16) for _ in range(2)] for _ in range(2)]
    NLFR = [ar.alloc([128], F32) for _ in range(2)]
    SMU = [[ar.alloc([128], BF16) for _ in range(2)] for _ in range(2)]
    EB = [[ar.alloc([128], F32) for _ in range(2)] for _ in range(2)]
    DEN = [ar.alloc([128], F32) for _ in range(2)]
    RDM = [ar.alloc([128], F32) for _ in range(2)]
    SQm = ar.alloc([512], BF16)
    SGm = ar.alloc([512], BF16)
    TMm = ar.alloc([512], F32)
    scale = 128.0 ** -0.5
    Wg_ = ar.alloc([8, 16], BF16)
    k.wload(Wg_, wcol(2816, 2832), "Wg_")
    b = k.ps_get()
    for t in range(18):
        for kk in range(8):
            k.mm(k.banks[b][:, t * 16:(t + 1) * 16], HT[:, kk, t * 128:(t + 1) * 128], Wg_[:, kk, :], kk == 0, kk == 7,
                 ["HT", "Wg_"], ["ps%d" % b])
    k.tt(DVE, GT, k.banks[b][:, 0:288].rearrange("p (a b) -> p a b", b=16),
         k.BG[:, ev * 16:(ev + 1) * 16].unsqueeze(1).to_broadcast([128, 18, 16]), ALU.add, ["ps%d" % b, "BG"], ["GT"])
    k.ps_put(b)
    for d in range(2):
        k.cp(DVE, IG[:, :, d * 4:(d + 1) * 4], GT[:, :, d * 8:d * 8 + 4], ["GT"], ["IG"])
        k.act(NLF[:, :, d * 4:(d + 1) * 4], GT[:, :, d * 8 + 4:d * 8 + 8], AF.Exp, ["GT"], ["NLF"], scale=-1.0)
    k.act(NLF, NLF, AF.Ln, ["NLF", "CSTF"], ["NLF"], bias=k.cf("onec", 1), scale=1.0)
    b = k.ps_get()
    for d in range(2):
        for j in range(18):
            k.mm(k.banks[b][:, j * 8 + d * 4:j * 8 + d * 4 + 4], k.cf("tril" if d == 0 else "triu"),
                 NLF[:, j, d * 4:(d + 1) * 4], True, True, ["NLF", "CSTF"], ["ps%d" % b])
    k.cp(DVE, NB, k.banks[b][:, 0:144].rearrange("p (a b) -> p a b", b=8), ["ps%d" % b], ["NB"])
    k.ps_put(b)
    b = k.ps_get()
    k.mm(k.banks[b][:, 0:144], k.cf("ones"), NLF.rearrange("p a b -> p (a b)"), True, True, ["NLF", "CSTF"], ["ps%d" % b])
    k.cp(DVE, NTOT, k.banks[b][:, 0:144].rearrange("p (a b) -> p a b", b=8), ["ps%d" % b], ["NTOT"])
    k.ps_put(b)
    k.tt(DVE, EXA, IG, NB, ALU.add, ["IG", "NB"], ["EXA"])
    k.act(US, EXA, AF.Exp, ["EXA"], ["US"])
    k.ts(DVE, US, US, scale, None, ALU.mult, None, ["US"], ["US"])
    k.tt(DVE, EXA, EXA, NTOT, ALU.subtract, ["EXA", "NTOT"], ["EXA"])
    k.act(WSC, EXA, AF.Exp, ["EXA"], ["WSC"])
    k.ts(DVE, WSC, WSC, scale, None, ALU.mult, None, ["WSC"], ["WSC"])
    k.act(DEC, NTOT, AF.Exp, ["NTOT"], ["DEC"], scale=-1.0)
    orders = [list(range(18)), [1, 0] + list(range(17, 1, -1))]
    k.P.op(POOL, lambda e: e.memset(XQ[:, :], 0.0), writes=["XQ"])
    for hd in range(4):
        HD = [CAT[:, hd, :], HDb]
        k.P.op(POOL, lambda e: e.memset(VB[:, :, 128:129], 1.0), writes=["VB"])
        Wv, wvk = None, None
        s = wbn[0] % 3
        wbn[0] += 1
        k.wload(WB[s], wcol(1792 + hd * 128, 1792 + (hd + 1) * 128), "WB%d" % s)
        Wv, wvk = WB[s], "WB%d" % s
        for t in range(18):
            b = k.ps_get()
            for kk in range(8):
                k.mm(k.banks[b][:, 0:128], HT[:, kk, t * 128:(t + 1) * 128], Wv[:, kk, :], kk == 0, kk == 7, ["HT", wvk],
                     ["ps%d" % b])
            k.cp(ACT if t % 2 else DVE, VB[:, t, 0:128], k.banks[b][:, 0:128], ["ps%d" % b], ["VB"])
            k.ps_put(b)
        for which, (c0, dest) in enumerate(((768 + hd * 128, QM), (1280 + hd * 128, KM))):
            s = wbn[0] % 3
            wbn[0] += 1
            k.wload(WB[s], wcol(c0, c0 + 128), "WB%d" % s)
            for (t0, t1) in BLOCKS:
                b = _proj_fm(k, WB[s], "WB%d" % s, HT, t0, t1)
                si = 0 if t0 < TC else 1
                off = qoff[si] + (t0 - SEGS[si][0])
                k.cp(ACT, XQ[:, off:off + (t1 - t0)], k.banks[b][:, 0:t1 - t0], ["ps%d" % b], ["XQ"])
                k.ps_put(b)
            cch = which * 4 + hd
            wc = lambda j: k.VEC[:, VO["wconv"] + (ev * 3 + j) * 8 + cch:VO["wconv"] + (ev * 3 + j) * 8 + cch + 1]
            for si, (s0, s1) in enumerate(SEGS):
                n = s1 - s0
                o = qoff[si]
                k.ts(DVE, ACC[:, s0:s1], XQ[:, o:o + n], wc(1), None, ALU.mult, None, ["XQ", "VEC"], ["ACC"])
                k.stt(ACC[:, s0:s1], XQ[:, o - 1:o - 1 + n], wc(0), ACC[:, s0:s1], ALU.mult, ALU.add, ["XQ", "VEC", "ACC"],
                      ["ACC"])
                k.stt(ACC[:, s0:s1], XQ[:, o + 1:o + 1 + n], wc(2), ACC[:, s0:s1], ALU.mult, ALU.add, ["XQ", "VEC", "ACC"],
                      ["ACC"])
            k.act(dest, ACC, AF.Silu, ["ACC"], ["QM" if which == 0 else "KM"])
        for d in range(2):
            k.P.op(POOL, lambda e, d=d: e.memset(CT[d][:, :], 0.0), writes=["CT%d" % d])
            k.P.op(POOL, lambda e, d=d: e.memset(CTB[d][0][:, :], 0.0), writes=["CTB%d0" % d])
            k.P.op(POOL, lambda e, d=d: e.memset(NREP[d][0][:, :], 0.0), writes=["NREP%d0" % d])
        bcl = {}

        def indep(step):
            for d in range(2):
                j = orders[d][step]
                gi = d * 4 + hd
                cols = slice(j * 128, (j + 1) * 128)
                pp = step % 2
                msk = k.cb("tril" if d == 0 else "triu")
                bs = k.ps_get()
                k.mm(k.banks[bs][:, 0:128], KM[:, cols], QM[:, cols], True, True, ["KM", "QM"], ["ps%d" % bs])
                k.stt(SMU[d][pp], k.banks[bs][:, 0:128], US[:, j, gi:gi + 1], msk, ALU.mult, ALU.mult,
                      ["ps%d" % bs, "US", "CSTB"], ["SMU%d%d" % (d, pp)])
                k.ps_put(bs)
                k.ts(POOL, NLFR[d], k.cf("ones"), NLF[:, j, gi:gi + 1], None, ALU.mult, None, ["CSTF", "NLF"], ["NLFR%d" % d])
                be = k.ps_get()
                k.mm(k.banks[be][:, 0:128], NLFR[d], k.cf("tril" if d == 0 else "triu"), True, True,
                     ["NLFR%d" % d, "CSTF"], ["ps%d" % be])
                k.act(EB[d][pp], k.banks[be][:, 0:128], AF.Exp, ["ps%d" % be], ["EB%d%d" % (d, pp)])
                k.ps_put(be)
                if step < 17:
                    bt = k.ps_get()
                    pv = k.banks[bt][:, 0:64].bitcast(BF16)
                    k.tr(pv, KM[:, cols], k.cb("ident"), ["KM", "CSTB"], ["ps%d" % bt])
                    k.act(KW[d][pp], pv, AF.Identity, ["ps%d" % bt, "WSC"], ["KW%d%d" % (d, pp)], scale=WSC[:, j, gi:gi + 1])
                    k.ps_put(bt)
                    bc = k.ps_get()
                    k.mm(k.banks[bc][:, 0:129], KW[d][pp], VB[:, j, :], True, True, ["KW%d%d" % (d, pp), "VB"], ["ps%d" % bc])
                    bcl[(step, d)] = bc

        def dep(step):
            pp = step % 2
            pn = (step + 1) % 2
            held = []
            for d in range(2):
                j = orders[d][step]
                gi = d * 4 + hd
                cols = slice(j * 128, (j + 1) * 128)
                bx = k.ps_get()
                k.mm(k.banks[bx][:, 0:128], VB[:, j, 0:128], SMU[d][pp], True, False, ["VB", "SMU%d%d" % (d, pp)], ["ps%d" % bx])
                k.mm(k.banks[bx][:, 0:128], CTB[d][pp], QM[:, cols], False, True, ["CTB%d%d" % (d, pp), "QM"], ["ps%d" % bx])
                by = k.ps_get()
                k.mm(k.banks[by][:, 0:128], k.cb("ones"), SMU[d][pp], True, False, ["CSTB", "SMU%d%d" % (d, pp)], ["ps%d" % by])
                k.mm(k.banks[by][:, 0:128], NREP[d][pp], QM[:, cols], False, True, ["NREP%d%d" % (d, pp), "QM"], ["ps%d" % by])
                held.append((d, j, cols, bx, by))
            if step < 17:
                for d in range(2):
                    j = orders[d][step]
                    gi = d * 4 + hd
                    bc = bcl.pop((step, d))
                    k.stt(CT[d], CT[d], DEC[:, j, gi:gi + 1], k.banks[bc][:, 0:129], ALU.mult, ALU.add,
                          ["CT%d" % d, "DEC", "ps%d" % bc], ["CT%d" % d])
                    k.ps_put(bc)
                    k.cp(POOL, CTB[d][pn], CT[d][:, 0:128], ["CT%d" % d], ["CTB%d%d" % (d, pn)])
                    k.ts(POOL, NREP[d][pn], k.cb("ones"), CT[d][:, 128:129], None, ALU.mult, None, ["CSTB", "CT%d" % d],
                         ["NREP%d%d" % (d, pn)])
            for (d, j, cols, bx, by) in held:
                k.tt(DVE, DEN[d], k.banks[by][:, 0:128], EB[d][pp], ALU.max, ["ps%d" % by, "EB%d%d" % (d, pp)], ["DEN%d" % d])
                k.stt(DEN[d], k.banks[by][:, 0:128], -1.0, DEN[d], ALU.mult, ALU.max, ["ps%d" % by, "DEN%d" % d],
                      ["DEN%d" % d])
                k.ps_put(by)
                k.act(RDM[d], DEN[d], AF.Ln, ["DEN%d" % d], ["RDM%d" % d])
                k.act(RDM[d], RDM[d], AF.Exp, ["RDM%d" % d], ["RDM%d" % d], scale=-1.0)
                k.tt(DVE, HD[d][:, cols], k.banks[bx][:, 0:128], RDM[d], ALU.mult, ["ps%d" % bx, "RDM%d" % d], ["HD%d" % d])
                k.ps_put(bx)

        indep(0)
        for step in range(18):
            if step + 1 < 18:
                indep(step + 1)
            dep(step)
        HMv = HM[:, 0:NT]
        k.tt(POOL, HMv, HD[0], HD[1], ALU.add, ["HD0", "HD1"], ["XQ"])
        s = wbn[0] % 3
        wbn[0] += 1
        k.wload(WB[s], wcol(2304 + hd * 128, 2304 + (hd + 1) * 128), "WB%d" % s)
        for (t0, t1) in oblocks:
            tw = t1 - t0
            k.act(SQm[:, 0:tw], HMv[:, t0:t1], AF.Square, ["XQ"], ["SQm"])
            b2 = k.ps_get()
            k.mm(k.banks[b2][:, 0:tw], k.cb("ones"), SQm[:, 0:tw], True, True, ["SQm", "CSTB"], ["ps%d" % b2])
            k.act(TMm[:, 0:tw], k.banks[b2][:, 0:tw], AF.Ln, ["ps%d" % b2, "CSTF"], ["TMm"], bias=k.cf("epsc", 1),
                  scale=1.0 / 128)
            k.ps_put(b2)
            k.act(TMm[:, 0:tw], TMm[:, 0:tw], AF.Exp, ["TMm"], ["TMm"], scale=-0.5)
            k.stt(TMm[:, 0:tw], HMv[:, t0:t1], k.VEC[:, VO["ghn"] + ev * 4 + hd:VO["ghn"] + ev * 4 + hd + 1], TMm[:, 0:tw],
                  ALU.mult, ALU.mult, ["XQ", "VEC", "TMm"], ["TMm"])
            b = _proj_fm(k, WB[s], "WB%d" % s, HT, t0, t1)
            k.act(SGm[:, 0:tw], k.banks[b][:, 0:tw], AF.Sigmoid, ["ps%d" % b], ["SGm"])
            k.ps_put(b)
            k.tt(DVE, CAT[:, hd, t0:t1], TMm[:, 0:tw], SGm[:, 0:tw], ALU.mult, ["TMm", "SGm"], ["CAT"])
        if hd < 3:
            k.P.op(POOL, lambda e: e.memset(XQ[:, :], 0.0), writes=["XQ"])
    P.barrier()
    ar.reset(mark_cat)
    out_proj(CAT, 1)
    P.barrier()
```

```python
from contextlib import ExitStack
import numpy as np
import concourse.bass as bass
import concourse.mybir as mybir
from concourse.bass_utils import run_bass_kernel_spmd

F32 = mybir.dt.float32
BF16 = mybir.dt.bfloat16
AF = mybir.ActivationFunctionType
ALU = mybir.AluOpType

PE, ACT, DVE, POOL, SP = "tensor", "scalar", "vector", "gpsimd", "sync"
ENGS = (PE, ACT, DVE, POOL, SP)
CENGS = (PE, ACT, DVE, POOL)

D = 1024
NT = 2304
TC = 256
TL = 2048
DEPTH = 4
NE = 16
EPS = 1e-6
CTX_NEXT = [True, True, False, False]
CTX_USED = [True, True, True, False]
BLOCKS = [(0, 256), (256, 768), (768, 1280), (1280, 1792), (1792, 2304)]
SEGS = [(0, 256), (256, 2304)]
POOL_W = (2, 4, 8, 16)

VO = {}
_o = 0
for _n, _c in (("bmod", 192), ("gn1", 32), ("gn2", 32), ("gfin", 8), ("spool", 16), ("wconv", 48),
               ("ghn", 8), ("gq", 2), ("gk", 2), ("cvec", 8), ("cctx", 8)):
    VO[_n] = _o
    _o += _c
NV = 384
CB = {"ident": 0, "ones": 128, "bd64": 256, "rperm": 384, "tril": 512, "triu": 640, "iotaf": 768, "sel": 1024}
NCB = 1024 + 2048
CF = {"ident": 0, "tril": 128, "triu": 256, "ones": 384, "iotac": 512, "epsc": 514, "onec": 515, "zeroc": 516}
NCF = 520


class Prog:
    def __init__(self, nc, stack):
        self.nc = nc
        self.stack = stack
        self.ops = {e: [] for e in ENGS}
        self.esem = {e: stack.enter_context(nc.semaphore("es_" + e)) for e in CENGS}
        self.ecount = {e: 0 for e in CENGS}
        self.seen = {e: {} for e in ENGS}
        self.lastw = {}
        self.readers = {}
        self.dsem = {}
        self.semobj = {}
        self.dcum = {}

    def _sem_for_key(self, key):
        if key not in self.dsem:
            s = self.stack.enter_context(self.nc.semaphore("ds%d" % len(self.dsem)))
            name = "ds_" + str(key)
            self.dsem[key] = [s, 0, name]
            self.semobj[name] = s
        return self.dsem[key]

    def _need(self, eng, tok, waits):
        if tok is None:
            return
        name, val = tok
        if name == "es_" + eng and eng == PE:
            return
        if name.startswith("ds_"):
            val = self.dcum[name]
        if self.seen[eng].get(name, 0) >= val:
            return
        self.seen[eng][name] = val
        waits[name] = max(waits.get(name, 0), val)

    def _wl(self, waits):
        wl = []
        for name, val in waits.items():
            sem = self.semobj[name] if name.startswith("ds_") else self.esem[name[3:]]
            wl.append((sem, val))
        return wl

    def op(self, eng, fn, reads=(), writes=(), dma_key=None, inc=True):
        waits = {}
        for k in reads:
            self._need(eng, self.lastw.get(k), waits)
        for k in writes:
            self._need(eng, self.lastw.get(k), waits)
            for t in self.readers.get(k, ()):
                self._need(eng, t, waits)
        if dma_key is not None:
            ent = self._sem_for_key(dma_key)
            ent[1] += 16
            self.dcum[ent[2]] = ent[1]
            tok = (ent[2], ent[1])
            inc = (ent[0], 16)
        elif eng == PE and not inc:
            tok = ("es_" + eng, self.ecount[eng] + 1)
            inc = None
        else:
            self.ecount[eng] += 1
            tok = ("es_" + eng, self.ecount[eng])
            inc = (self.esem[eng], 1)
        self.ops[eng].append((self._wl(waits), fn, inc))
        for k in writes:
            self.lastw[k] = tok
            self.readers[k] = []
        for k in reads:
            self.readers.setdefault(k, []).append(tok)
        return tok

    def barrier(self):
        for e in ENGS:
            waits = {}
            for o in CENGS:
                if self.ecount[o]:
                    self._need(e, ("es_" + o, self.ecount[o]), waits)
            for key, ent in self.dsem.items():
                if ent[1]:
                    self._need(e, (ent[2], ent[1]), waits)
            self.ops[e].append((self._wl(waits), None, None))
        self.lastw = {}
        self.readers = {}

    def emit(self):
        with self.nc.Block() as block:
            def mk(e):
                def body(engine):
                    for wl, fn, inc in self.ops[e]:
                        for sem, val in wl:
                            engine.wait_ge(sem, val)
                        if fn is not None:
                            ins = fn(engine)
                            if inc is not None:
                                ins.then_inc(inc[0], inc[1])
                return body
            block.tensor(mk(PE))
            block.scalar(mk(ACT))
            block.vector(mk(DVE))
            block.gpsimd(mk(POOL))
            block.sync(mk(SP))


class Arena:
    def __init__(self, ap, nwords):
        self.ap = ap
        self.n = nwords
        self.off = 0

    def reset(self, off=0):
        self.off = off

    def alloc(self, shape, dtype):
        nel = int(np.prod(shape))
        words = nel if dtype == F32 else (nel + 1) // 2
        words += words & 1
        a = self.ap[:, self.off:self.off + words]
        self.off += words
        assert self.off <= self.n, ("arena overflow", self.off, self.n)
        if dtype == BF16:
            a = a.bitcast(BF16)
        a = a[:, 0:nel]
        if len(shape) == 2:
            a = a.rearrange("p (a b) -> p a b", a=shape[0])
        elif len(shape) == 3:
            a = a.rearrange("p (a b c) -> p a b c", a=shape[0], b=shape[1])
        return a


class K:
    def __init__(self, nc, st, dr):
        self.nc = nc
        self.P = Prog(nc, st)
        self.dr = dr
        self.uid = 0
        sb = lambda n, s, d: st.enter_context(nc.sbuf_tensor(n, s, d))
        self.XT = sb("XT", [128, 8, NT], F32)
        self.CSTB = sb("CSTB", [128, NCB], BF16)
        self.CSTF = sb("CSTF", [128, NCF], F32)
        self.VEC = sb("VEC", [128, NV], F32)
        self.MODX = sb("MODX", [128, DEPTH, 2, 6, 8], F32)
        self.BG = sb("BG", [128, 32], F32)
        self.AR_WORDS = 30400
        self.ARENA = sb("ARENA", [128, self.AR_WORDS], F32)
        self.ar = Arena(self.ARENA, self.AR_WORDS)
        self.banks = [st.enter_context(nc.psum_tensor("pb%d" % i, [128, 512], F32)) for i in range(8)]
        self.bank_lru = list(range(8))

    def key(self, s):
        self.uid += 1
        return "%s#%d" % (s, self.uid)

    def ps_get(self):
        b = self.bank_lru.pop(0)
        return b

    def ps_put(self, b):
        self.bank_lru.append(b)

    def cb(self, name, w=128, rows=128):
        o = CB[name]
        return self.CSTB[0:rows, o:o + w]

    def cf(self, name, w=128, rows=128):
        o = CF[name]
        return self.CSTF[0:rows, o:o + w]

    def mm(self, out, lhsT, rhs, start, stop, r, w):
        self.P.op(PE, lambda e: e.matmul(out, lhsT=lhsT, rhs=rhs, start=start, stop=stop), reads=r, writes=w, inc=bool(stop))

    def tr(self, out, in_, ident, r, w):
        self.P.op(PE, lambda e: e.transpose(out, in_, ident), reads=r, writes=w)

    def act(self, out, in_, func, r, w, bias=None, scale=None, accum_out=None):
        kw = {}
        if accum_out is not None:
            kw["accum_out"] = accum_out
        if bias is not None:
            kw["bias"] = bias
        if scale is not None:
            kw["scale"] = scale
        self.P.op(ACT, lambda e: e.activation(out, in_, func, **kw), reads=r, writes=w)

    def tt(self, eng, out, in0, in1, op, r, w):
        self.P.op(eng, lambda e: e.tensor_tensor(out, in0, in1, op), reads=r, writes=w)

    def ts(self, eng, out, in0, s1, s2, op0, op1, r, w):
        if op1 is None:
            self.P.op(eng, lambda e: e.tensor_scalar(out, in0, s1, None, op0), reads=r, writes=w)
        else:
            self.P.op(eng, lambda e: e.tensor_scalar(out, in0, s1, s2, op0, op1), reads=r, writes=w)

    def stt(self, out, in0, scalar, in1, op0, op1, r, w):
        self.P.op(DVE, lambda e: e.scalar_tensor_tensor(out, in0, scalar, in1, op0, op1), reads=r, writes=w)

    def cp(self, eng, out, in_, r, w):
        if eng == ACT:
            self.P.op(ACT, lambda e: e.copy(out, in_), reads=r, writes=w)
        else:
            self.P.op(eng, lambda e: e.tensor_copy(out, in_), reads=r, writes=w)

    def dma(self, eng, out, in_, r, w, key):
        self.P.op(eng, lambda e: e.dma_start(out=out, in_=in_), reads=r, writes=w, dma_key=key)

    def wload(self, dst, src, key, nparts=1):
        self.dma(POOL, dst, src, [], [key], key)


def build(dbg=False, stop_after=None):
    nc = bass.Bass("TRN2", target_bir_lowering=False)
    dt = lambda n, s, kind="ExternalInput": nc.dram_tensor(n, s, F32, kind=kind).ap()
    dr = {
        "xin": dt("xin", [NT, D]), "vecs": dt("vecs", [NV, 128]), "cstb": dt("cstb", [128, NCB]),
        "cstf": dt("cstf", [128, NCF]), "rope": dt("rope", [2, 128, TL]), "bgate": dt("bgate", [128, 32]),
        "invcnt": dt("invcnt", [4, 128, NT]),
        "w_mod": dt("w_mod", [DEPTH, D, 6 * D]), "w_in_even": dt("w_in_even", [2, D, 2832]),
        "w_out_even": dt("w_out_even", [2, D, D]), "w_in_odd": dt("w_in_odd", [2, D, D]),
        "w_pool_grp": dt("w_pool_grp", [2, 4, 256, 256]), "w_router": dt("w_router", [DEPTH, D, NE]),
        "w_exp_gate": dt("w_exp_gate", [DEPTH, NE, D, D]), "w_exp_up": dt("w_exp_up", [DEPTH, NE, D, D]),
        "w_exp_down": dt("w_exp_down", [DEPTH, NE, D, D]),
        "out": dt("out", [TL, D], kind="ExternalOutput"),
    }
    if dbg:
        dr["dbg"] = dt("dbg", [8, 128, 8, NT], kind="ExternalOutput")
    with ExitStack() as st:
        k = K(nc, st, dr)
        phase_init(k)
        if stop_after != ("init", 0):
            phase_adaln(k)
        for i in range(DEPTH):
            if stop_after in (("init", 0), ("adaln", 0)):
                if dbg:
                    dump(k, 0)
                break
            if i % 2 == 0:
                phase_even(k, i)
            else:
                phase_odd(k, i)
            if dbg:
                dump(k, 2 * i)
            if stop_after == ("mix", i):
                done = True
                break
            phase_moe(k, i)
            if dbg:
                dump(k, 2 * i + 1)
            if stop_after == ("moe", i):
                done = True
                break
        phase_final(k)
        k.P.emit()
    return nc


def dump(k, slot):
    k.dma(SP, k.dr["dbg"][slot], k.XT[:, :, :], ["XT"], [], "dbg")
    k.P.barrier()


def phase_init(k):
    P, dr, ar = k.P, k.dr, k.ar
    ar.reset()
    for h in range(2):
        k.dma(POOL, k.CSTB[:, h * 1536:(h + 1) * 1536], dr["cstb"][:, h * 1536:(h + 1) * 1536], [], ["CSTB"], "CSTB")
    k.dma(SP, k.CSTF[:, :], dr["cstf"][:, :], [], ["CSTF"], "CSTF")
    k.dma(SP, k.BG[:, :], dr["bgate"][:, :], [], ["BG"], "BG")
    vst = ar.alloc([3, 128], F32)
    k.dma(SP, vst, dr["vecs"].rearrange("(a p) n -> p a n", p=128), [], ["vst"], "vst")
    b = k.ps_get()
    pk = "ps%d" % b
    for a in range(3):
        k.tr(k.banks[b][:, a * 128:(a + 1) * 128], vst[:, a, :], k.cf("ident"), ["vst", "CSTF"], [pk])
    k.cp(DVE, k.VEC[:, :], k.banks[b][:, 0:384], [pk], ["VEC"])
    k.ps_put(b)
    xst = [ar.alloc([1024], F32) for _ in range(2)]
    for t in range(18):
        s = xst[t % 2]
        sk = "xst%d" % (t % 2)
        k.dma(SP if t % 2 == 0 else ACT, s, dr["xin"][t * 128:(t + 1) * 128, :], [], [sk], sk)
        for half in range(2):
            b = k.ps_get()
            pk = "ps%d" % b
            for j in range(4):
                c = half * 4 + j
                k.tr(k.banks[b][:, j * 128:(j + 1) * 128], s[:, c * 128:(c + 1) * 128], k.cf("ident"),
                     [sk, "CSTF"], [pk])
            src = k.banks[b][:, :].rearrange("p (a b) -> p a b", a=4)
            k.cp(DVE if half == 0 else ACT, k.XT[:, half * 4:half * 4 + 4, t * 128:(t + 1) * 128], src, [pk], ["XT"])
            k.ps_put(b)
    P.barrier()


def phase_adaln(k):
    P, dr, ar = k.P, k.dr, k.ar
    ar.reset()
    WM = [ar.alloc([8, 1024], BF16) for _ in range(2)]
    SCf = ar.alloc([8, 2], F32)
    SC = ar.alloc([8, 2], BF16)
    MODR = ar.alloc([DEPTH, 48, 2], F32)
    T1 = ar.alloc([8], F32)
    for w, nm in enumerate(("cvec", "cctx")):
        k.act(SCf[:, :, w], k.VEC[:, VO[nm]:VO[nm] + 8], AF.Silu, ["VEC"], ["SCf"])
    k.cp(DVE, SC, SCf, ["SCf"], ["SC"])
    n = 0
    for i in range(DEPTH):
        b = k.ps_get()
        pk = "ps%d" % b
        for j in range(6):
            wk = "WM%d" % (n % 2)
            Wb = WM[n % 2]
            n += 1
            k.wload(Wb, dr["w_mod"][i][:, j * 1024:(j + 1) * 1024].rearrange("(c p) n -> p c n", p=128), wk)
            for f in range(8):
                col = (j * 8 + f) * 2
                for kk in range(8):
                    k.mm(k.banks[b][:, col:col + 2], Wb[:, kk, f * 128:(f + 1) * 128], SC[:, kk, :],
                         kk == 0, kk == 7, [wk, "SC"], [pk])
        src = k.banks[b][:, 0:96].rearrange("p (a b) -> p a b", b=2)
        for w in range(2):
            k.tt(DVE, MODR[:, i, :, w], src[:, :, w], k.VEC[:, VO["bmod"] + i * 48:VO["bmod"] + (i + 1) * 48],
                 ALU.add, [pk, "VEC"], ["MODR"])
        k.ps_put(b)
        for w in range(2):
            m = lambda j: MODR[:, i, j * 8:(j + 1) * 8, w]
            MX = lambda q: k.MODX[:, i, w, q, :]
            for (q, jscale, gname) in ((0, 1, "gn1"), (3, 4, "gn2")):
                k.ts(DVE, T1, m(jscale), 1.0, None, ALU.add, None, ["MODR"], ["T1"])
                k.tt(DVE, MX(q), T1, k.VEC[:, VO[gname] + i * 8:VO[gname] + (i + 1) * 8], ALU.mult,
                     ["T1", "VEC"], ["MODX"])
            k.cp(DVE, MX(1), m(0), ["MODR"], ["MODX"])
            k.cp(DVE, MX(4), m(3), ["MODR"], ["MODX"])
            k.cp(DVE, MX(5), m(5), ["MODR"], ["MODX"])
            if i % 2 == 0:
                k.cp(DVE, MX(2), m(2), ["MODR"], ["MODX"])
            else:
                o = i // 2
                k.tt(DVE, MX(2), m(2), k.VEC[:, VO["spool"] + o * 8:VO["spool"] + (o + 1) * 8], ALU.mult,
                     ["MODR", "VEC"], ["MODX"])
    P.barrier()


def norm_mod(k, i, which, HT, include_ctx=True):
    ar = k.ar
    SQ = [ar.alloc([8, 512], BF16) for _ in range(2)]
    RS = [ar.alloc([512], F32) for _ in range(2)]
    LN = ar.alloc([512], F32)
    TMP = [ar.alloc([512], F32) for _ in range(2)]
    qg, qs = (0, 1) if which == 0 else (3, 4)
    n = 0
    for bi, (t0, t1) in enumerate(BLOCKS):
        if bi == 0 and not include_ctx:
            continue
        w = 1 if bi == 0 else 0
        tw = t1 - t0
        sq = SQ[bi % 2]
        sqk = "SQ%d" % (bi % 2)
        rs = RS[bi % 2]
        rsk = "RS%d" % (bi % 2)
        k.act(sq[:, :, 0:tw], k.XT[:, :, t0:t1], AF.Square, ["XT"], [sqk])
        b = k.ps_get()
        pk = "ps%d" % b
        for c in range(8):
            k.mm(k.banks[b][:, 0:tw], k.cb("ones"), sq[:, c, 0:tw], c == 0, c == 7, [sqk, "CSTB"], [pk])
        k.act(LN[:, 0:tw], k.banks[b][:, 0:tw], AF.Ln, [pk, "CSTF"], ["LN"], bias=k.cf("epsc", 1), scale=1.0 / D)
        k.ps_put(b)
        k.act(rs[:, 0:tw], LN[:, 0:tw], AF.Exp, ["LN"], [rsk], scale=-0.5)
        for c in range(8):
            tmp = TMP[n % 2]
            tk = "TMPn%d" % (n % 2)
            n += 1
            k.stt(tmp[:, 0:tw], k.XT[:, c, t0:t1], k.MODX[:, i, w, qg, c:c + 1], rs[:, 0:tw], ALU.mult, ALU.mult,
                  ["XT", "MODX", rsk], [tk])
            k.act(HT[:, c, t0:t1], tmp[:, 0:tw], AF.Identity, [tk, "MODX"], ["HT"],
                  bias=k.MODX[:, i, w, qs, c:c + 1], scale=1.0)


def phase_odd(k, i):
    P, dr, ar = k.P, k.dr, k.ar
    o = i // 2
    ar.reset()
    inc_ctx = CTX_NEXT[i]
    HT = ar.alloc([8, NT], BF16)
    mark = ar.off
    norm_mod(k, i, 0, HT, include_ctx=inc_ctx)
    P.barrier()
    ar.reset(mark)
    WIN = ar.alloc([8, 1024], BF16)
    WG = ar.alloc([4, 2, 256], BF16)
    PADL, PADR = 16, 8
    UW = PADL + TC + PADR + PADL + TL + PADR
    segoff = [PADL, PADL + TC + PADR + PADL]
    U = [ar.alloc([UW], F32) for _ in range(2)]
    AB = [ar.alloc([UW], F32) for _ in range(2)]
    IC = ar.alloc([NT], F32)
    PP = [ar.alloc([NT], BF16) for _ in range(2)]
    k.wload(WIN, dr["w_in_odd"][o].rearrange("(c p) n -> p c n", p=128), "WIN")
    for g in range(4):
        k.wload(WG[:, g], dr["w_pool_grp"][o][g].rearrange("(c p) n -> p c n", p=128), "WG")
    for j in range(2):
        k.P.op(POOL, lambda e, j=j: e.memset(U[j][:, :], 0.0), writes=["U%d" % j])
        k.P.op(POOL, lambda e, j=j: e.memset(AB[j][:, :], 0.0), writes=["AB%d" % j])
    blocks = BLOCKS if inc_ctx else BLOCKS[1:]
    segs = [0, 1] if inc_ctx else [1]
    for g in range(4):
        wnd = POOL_W[g]
        lo = wnd // 2
        hi = wnd - 1 - lo
        k.dma(SP, IC, dr["invcnt"][g], [], ["IC"], "IC")
        for j in range(2):
            cc = 2 * g + j
            uk = "U%d" % j
            for (t0, t1) in blocks:
                tw = t1 - t0
                b = k.ps_get()
                pk = "ps%d" % b
                for kk in range(8):
                    k.mm(k.banks[b][:, 0:tw], WIN[:, kk, cc * 128:(cc + 1) * 128], HT[:, kk, t0:t1], kk == 0, kk == 7,
                         ["WIN", "HT"], [pk])
                si = 0 if t0 < TC else 1
                off = segoff[si] + (t0 - SEGS[si][0])
                k.cp(ACT, U[j][:, off:off + tw], k.banks[b][:, 0:tw], [pk], [uk])
                k.ps_put(b)
            src, srck = U[j], uk
            nd = 0
            step = 1
            while step < wnd:
                dst, dstk = AB[nd % 2], "AB%d" % (nd % 2)
                k.tt(POOL, dst[:, PADL:UW], src[:, PADL:UW], src[:, PADL - step:UW - step], ALU.add, [srck], [dstk])
                src, srck = dst, dstk
                nd += 1
                step *= 2
            oth, othk = AB[nd % 2], "AB%d" % (nd % 2)
            for si in segs:
                s0, s1 = SEGS[si]
                n = s1 - s0
                so = segoff[si]
                k.tt(DVE, oth[:, so:so + n], src[:, so + hi:so + hi + n], IC[:, s0:s1], ALU.mult, [srck, "IC"], [othk])
                k.tt(DVE, PP[j][:, s0:s1], oth[:, so:so + n], U[j][:, so:so + n], ALU.subtract, [othk, uk], ["PP%d" % j])
        for jj in range(2):
            ec = 2 * g + jj
            for (t0, t1) in blocks:
                tw = t1 - t0
                w = 1 if t0 < TC else 0
                b = k.ps_get()
                pk = "ps%d" % b
                for kc in range(2):
                    k.mm(k.banks[b][:, 0:tw], WG[:, g, kc, jj * 128:(jj + 1) * 128], PP[kc][:, t0:t1], kc == 0, kc == 1,
                         ["WG", "PP%d" % kc], [pk])
                k.stt(k.XT[:, ec, t0:t1], k.banks[b][:, 0:tw], k.MODX[:, i, w, 2, ec:ec + 1], k.XT[:, ec, t0:t1],
                      ALU.mult, ALU.add, [pk, "MODX", "XT"], ["XT"])
                k.ps_put(b)
    P.barrier()


def phase_moe(k, i):
    P, dr, ar = k.P, k.dr, k.ar
    do_ctx = CTX_NEXT[i]
    ar.reset()
    HTOK = ar.alloc([18, 1024], BF16)
    AFT = ar.alloc([18, 16], F32)
    AFTB = ar.alloc([18, 16], BF16)
    POST = ar.alloc([18, 16], F32)
    POSB = ar.alloc([NT], BF16)
    GSB = ar.alloc([4], F32)
    M8 = ar.alloc([8], F32)
    mark_small = ar.off
    HT = ar.alloc([8, NT], BF16)
    mark_ht = ar.off
    norm_mod(k, i, 1, HT, include_ctx=do_ctx)
    P.barrier()
    ar.reset(mark_ht)
    tiles = list(range(18)) if do_ctx else list(range(2, 18))
    WR = ar.alloc([8, 16], BF16)
    ET = ar.alloc([18, 16], F32)
    Z = ar.alloc([18], F32)
    RZ = ar.alloc([18], F32)
    k.wload(WR, dr["w_router"][i].rearrange("(c p) n -> p c n", p=128), "WR")
    b = k.ps_get()
    pk = "ps%d" % b
    for t in tiles:
        for c in range(8):
            k.mm(k.banks[b][:, t * 16:(t + 1) * 16], HT[:, c, t * 128:(t + 1) * 128], WR[:, c, :], c == 0, c == 7,
                 ["HT", "WR"], [pk])
    t0, t1 = tiles[0], tiles[-1] + 1
    k.act(ET[:, t0:t1, :], k.banks[b][:, t0 * 16:t1 * 16].rearrange("p (a b) -> p a b", b=16), AF.Exp, [pk], ["ET"])
    k.ps_put(b)
    k.P.op(DVE, lambda e: e.tensor_reduce(Z[:, t0:t1], ET[:, t0:t1, :], mybir.AxisListType.X, ALU.add),
           reads=["ET"], writes=["Z"])
    k.P.op(DVE, lambda e: e.reciprocal(RZ[:, t0:t1], Z[:, t0:t1]), reads=["Z"], writes=["RZ"])
    for t in tiles:
        k.ts(DVE, AFT[:, t, :], ET[:, t, :], RZ[:, t:t + 1], None, ALU.mult, None, ["ET", "RZ"], ["AFT"])
    k.cp(DVE, AFTB[:, t0:t1, :], AFT[:, t0:t1, :], ["AFT"], ["AFTB"])
    n = 0
    for t in tiles:
        for half in range(2):
            b = k.ps_get()
            pk = "ps%d" % b
            pv = k.banks[b][:, 0:256].bitcast(BF16)
            for j in range(4):
                c = half * 4 + j
                k.tr(pv[:, j * 128:(j + 1) * 128], HT[:, c, t * 128:(t + 1) * 128], k.cb("ident"), ["HT", "CSTB"], [pk])
            k.cp(DVE if n % 2 == 0 else ACT, HTOK[:, t, half * 512:(half + 1) * 512], pv, [pk], ["HTOK"])
            n += 1
            k.ps_put(b)
    P.barrier()
    ar.reset(mark_small)
    AFF = ar.alloc([NT], F32)
    WORK = ar.alloc([NT], F32)
    b = k.ps_get()
    pk = "ps%d" % b
    nb = 0
    for t in tiles:
        k.tr(k.banks[b][0:16, nb * 128:(nb + 1) * 128], AFT[:, t, :], k.cf("ident"), ["AFT", "CSTF"], [pk])
        nb += 1
        if nb == 4 or t == tiles[-1]:
            ts0 = (t - nb + 1) * 128
            k.cp(ACT, AFF[0:16, ts0:ts0 + nb * 128], k.banks[b][0:16, 0:nb * 128], [pk], ["AFF"])
            k.ps_put(b)
            if t != tiles[-1]:
                b = k.ps_get()
                pk = "ps%d" % b
            nb = 0
    segl = [(TC, NT, 256)] + ([(0, TC, 32)] if do_ctx else [])
    for (s0, s1, cap) in segl:
        wk_, af_, m8_ = WORK[0:16, s0:s1], AFF[0:16, s0:s1], M8[0:16, :]
        zb_ = k.cf("zeroc", 1)[0:16, :].to_broadcast([16, s1 - s0])
        k.cp(DVE, wk_, af_, ["AFF"], ["WORK"])
        rounds = cap // 8
        for r in range(rounds):
            k.P.op(DVE, lambda e, wk_=wk_, m8_=m8_: e.max(m8_, wk_), reads=["WORK"], writes=["M8"])
            if r < rounds - 1:
                k.P.op(DVE, lambda e, wk_=wk_, m8_=m8_: e.match_replace(wk_, m8_, wk_, -1.0),
                       reads=["WORK", "M8"], writes=["WORK"])
        k.ts(DVE, wk_, af_, M8[0:16, 7:8], None, ALU.is_ge, None, ["AFF", "M8"], ["WORK"])
        k.P.op(DVE, lambda e, wk_=wk_, af_=af_, zb_=zb_: e.tensor_tensor_scan(af_, wk_, zb_, 0.0, ALU.add, ALU.add),
               reads=["WORK", "CSTF"], writes=["AFF"])
        k.tt(DVE, af_, af_, wk_, ALU.mult, ["AFF", "WORK"], ["AFF"])
        k.ts(DVE, af_, af_, -1.0, None, ALU.add, None, ["AFF"], ["AFF"])
        k.cp(DVE, POSB[0:16, s0:s1], af_, ["AFF"], ["POSB"])
    b = k.ps_get()
    pk = "ps%d" % b
    for t in tiles:
        k.tr(k.banks[b][:, t * 16:(t + 1) * 16], AFF[0:16, t * 128:(t + 1) * 128], k.cf("ident", 16, 16),
             ["AFF", "CSTF"], [pk])
    k.cp(DVE, POST[:, t0:t1, :], k.banks[b][:, t0 * 16:t1 * 16].rearrange("p (a b) -> p a b", b=16), [pk], ["POST"])
    k.ps_put(b)
    P.barrier()
    ar.reset(mark_small)
    NS = 288 if do_ctx else 256
    PEHs = [ar.alloc([16, 256], BF16) for _ in range(2)]
    PECs = [ar.alloc([2, 32], BF16) for _ in range(2)]
    XS = ar.alloc([8, 288], BF16)
    A = ar.alloc([8, 288], BF16)
    SGT = [ar.alloc([288], F32) for _ in range(2)]
    YSB = [ar.alloc([2, 1024], BF16) for _ in range(2)]
    YSC = [ar.alloc([1024], BF16) for _ in range(2)]
    PT = [ar.alloc([2, TL], BF16) for _ in range(2)]
    PTC = [ar.alloc([TC], BF16) for _ in range(2)]
    NSLOT = 4
    WS = [ar.alloc([2048], BF16) for _ in range(NSLOT)]
    wn = [0]

    def wq(src_ap, shape_a):
        s = wn[0] % NSLOT
        wn[0] += 1
        v = WS[s].rearrange("p (a b) -> p a b", a=shape_a)
        k.wload(v, src_ap, "WS%d" % s)
        return v, "WS%d" % s

    def build_onehot(e):
        pb = e % 2
        for t in range(16):
            k.ts(DVE, PEHs[pb][:, t, :], k.cb("iotaf", 256), POST[:, 2 + t, e:e + 1], None,
                 ALU.is_equal, None, ["CSTB", "POST"], ["PEH%d_%d" % (pb, t)])
        if do_ctx:
            for t in range(2):
                k.ts(DVE, PECs[pb][:, t, :], k.cb("iotaf", 32), POST[:, t, e:e + 1], None, ALU.is_equal, None,
                     ["CSTB", "POST"], ["PEC%d" % pb])

    build_onehot(0)
    for e in range(NE):
        slot = e % 2
        PEH, PEC = PEHs[e % 2], PECs[e % 2]
        pehk = lambda t: "PEH%d_%d" % (e % 2, t)
        peck = "PEC%d" % (e % 2)
        b = k.ps_get()
        pk = "ps%d" % b
        for ct in range(2):
            for t in range(16):
                k.mm(k.banks[b][:, ct:ct + 1], PEH[:, t, ct * 128:(ct + 1) * 128], AFTB[:, 2 + t, e:e + 1], t == 0, t == 15,
                     [pehk(t), "AFTB"], [pk])
        if do_ctx:
            for t in range(2):
                k.mm(k.banks[b][0:32, 2:3], PEC[:, t, :], AFTB[:, t, e:e + 1], t == 0, t == 1, [peck, "AFTB"], [pk])
        k.cp(DVE, GSB[:, 0:3], k.banks[b][:, 0:3], [pk], ["GSB"])
        k.ps_put(b)
        for dc in range(8):
            b = k.ps_get()
            pk = "ps%d" % b
            for t in range(16):
                k.mm(k.banks[b][:, 0:256], HTOK[:, 2 + t, dc * 128:(dc + 1) * 128], PEH[:, t, :], t == 0, t == 15,
                     ["HTOK", pehk(t)], [pk])
            if do_ctx:
                for t in range(2):
                    k.mm(k.banks[b][:, 256:288], HTOK[:, t, dc * 128:(dc + 1) * 128], PEC[:, t, :], t == 0, t == 1,
                         ["HTOK", peck], [pk])
            k.cp(ACT, XS[:, dc, 0:NS], k.banks[b][:, 0:NS], [pk], ["XS"])
            k.ps_put(b)
        for q in range(4):
            wg, wgk = wq(dr["w_exp_gate"][i, e][:, q * 256:(q + 1) * 256].rearrange("(c p) n -> p c n", p=128), 8)
            wu, wuk = wq(dr["w_exp_up"][i, e][:, q * 256:(q + 1) * 256].rearrange("(c p) n -> p c n", p=128), 8)
            for ff in range(2):
                f = q * 2 + ff
                bg = k.ps_get()
                bu = k.ps_get()
                for kk in range(8):
                    k.mm(k.banks[bg][:, 0:NS], wg[:, kk, ff * 128:(ff + 1) * 128], XS[:, kk, 0:NS], kk == 0, kk == 7,
                         [wgk, "XS"], ["ps%d" % bg])
                for kk in range(8):
                    k.mm(k.banks[bu][:, 0:NS], wu[:, kk, ff * 128:(ff + 1) * 128], XS[:, kk, 0:NS], kk == 0, kk == 7,
                         [wuk, "XS"], ["ps%d" % bu])
                sg = SGT[f % 2]
                sgk = "SGT%d" % (f % 2)
                k.act(sg[:, 0:NS], k.banks[bg][:, 0:NS], AF.Silu, ["ps%d" % bg], [sgk])
                k.tt(DVE, A[:, f, 0:NS], sg[:, 0:NS], k.banks[bu][:, 0:NS], ALU.mult, [sgk, "ps%d" % bu], ["A"])
                k.ps_put(bg)
                k.ps_put(bu)
        if e + 1 < NE:
            build_onehot(e + 1)
        ybanks = [k.ps_get() for _ in range(4)]
        cbanks = [k.ps_get() for _ in range(2)] if do_ctx else []
        for q in range(4):
            wd, wdk = wq(dr["w_exp_down"][i, e][q * 256:(q + 1) * 256, :].rearrange("(c p) n -> p c n", p=128), 2)
            for ff in range(2):
                f = q * 2 + ff
                for ct in range(2):
                    for half in range(2):
                        bb = ybanks[ct * 2 + half]
                        k.mm(k.banks[bb][:, :], A[:, f, ct * 128:(ct + 1) * 128], wd[:, ff, half * 512:(half + 1) * 512],
                             f == 0, f == 7, ["A", wdk], ["ps%d" % bb])
                if do_ctx:
                    for half in range(2):
                        bb = cbanks[half]
                        k.mm(k.banks[bb][0:32, :], A[:, f, 256:288], wd[:, ff, half * 512:(half + 1) * 512],
                             f == 0, f == 7, ["A", wdk], ["ps%d" % bb])
        for ct in range(2):
            for half in range(2):
                bb = ybanks[ct * 2 + half]
                k.act(YSB[slot][:, ct, half * 512:(half + 1) * 512], k.banks[bb][:, :], AF.Identity, ["ps%d" % bb, "GSB"],
                      ["YSB%d" % slot], scale=GSB[:, ct:ct + 1])
                k.ps_put(bb)
        if do_ctx:
            for half in range(2):
                bb = cbanks[half]
                k.act(YSC[slot][0:32, half * 512:(half + 1) * 512], k.banks[bb][0:32, :], AF.Identity, ["ps%d" % bb, "GSB"],
                      ["YSC%d" % slot], scale=GSB[0:32, 2:3])
                k.ps_put(bb)
        sel = k.CSTB[0:16, CB["sel"] + e * 128:CB["sel"] + (e + 1) * 128]
        for tb in range(4):
            b = k.ps_get()
            pk = "ps%d" % b
            k.mm(k.banks[b][:, :], sel, POSB[0:16, TC + tb * 512:TC + (tb + 1) * 512], True, True, ["CSTB", "POSB"], [pk])
            for ct in range(2):
                k.ts(DVE, PT[slot][:, ct, tb * 512:(tb + 1) * 512], k.banks[b][:, :], k.cf("iotac", 2)[:, ct:ct + 1], None,
                     ALU.is_equal, None, [pk, "CSTF"], ["PT%d" % slot])
            k.ps_put(b)
        if do_ctx:
            b = k.ps_get()
            pk = "ps%d" % b
            k.mm(k.banks[b][:, 0:TC], sel, POSB[0:16, 0:TC], True, True, ["CSTB", "POSB"], [pk])
            k.ts(DVE, PTC[slot][0:32, :], k.banks[b][0:32, 0:TC], k.cf("iotac", 2)[0:32, 0:1], None, ALU.is_equal, None,
                 [pk, "CSTF"], ["PTC%d" % slot])
            k.ps_put(b)
        if slot == 1:
            for dmc in range(8):
                for tb in range(4):
                    b = k.ps_get()
                    pk = "ps%d" % b
                    nmm = 0
                    for s in range(2):
                        for ct in range(2):
                            k.mm(k.banks[b][:, :], YSB[s][:, ct, dmc * 128:(dmc + 1) * 128],
                                 PT[s][:, ct, tb * 512:(tb + 1) * 512], nmm == 0, nmm == 3, ["YSB%d" % s, "PT%d" % s], [pk])
                            nmm += 1
                    xs = k.XT[:, dmc, TC + tb * 512:TC + (tb + 1) * 512]
                    k.stt(xs, k.banks[b][:, :], k.MODX[:, i, 0, 5, dmc:dmc + 1], xs, ALU.mult, ALU.add,
                          [pk, "MODX", "XT"], ["XT"])
                    k.ps_put(b)
                if do_ctx:
                    b = k.ps_get()
                    pk = "ps%d" % b
                    for s in range(2):
                        k.mm(k.banks[b][:, 0:TC], YSC[s][0:32, dmc * 128:(dmc + 1) * 128], PTC[s][0:32, :], s == 0, s == 1,
                             ["YSC%d" % s, "PTC%d" % s], [pk])
                    xs = k.XT[:, dmc, 0:TC]
                    k.stt(xs, k.banks[b][:, 0:TC], k.MODX[:, i, 1, 5, dmc:dmc + 1], xs, ALU.mult, ALU.add,
                          [pk, "MODX", "XT"], ["XT"])
                    k.ps_put(b)
    P.barrier()


def phase_final(k):
    P, dr, ar = k.P, k.dr, k.ar
    ar.reset()
    SQ = [ar.alloc([8, 512], BF16) for _ in range(2)]
    RS = [ar.alloc([512], F32) for _ in range(2)]
    LN = ar.alloc([512], F32)
    YT = [ar.alloc([8, 512], F32) for _ in range(2)]
    OST = [ar.alloc([1024], F32) for _ in range(2)]
    n = 0
    for bi, (t0, t1) in enumerate(BLOCKS[1:]):
        sq, sqk = SQ[bi % 2], "SQ%d" % (bi % 2)
        rs, rsk = RS[bi % 2], "RS%d" % (bi % 2)
        yt, ytk = YT[bi % 2], "YT%d" % (bi % 2)
        k.act(sq, k.XT[:, :, t0:t1], AF.Square, ["XT"], [sqk])
        b = k.ps_get()
        pk = "ps%d" % b
        for c in range(8):
            k.mm(k.banks[b][:, :], k.cb("ones"), sq[:, c, :], c == 0, c == 7, [sqk, "CSTB"], [pk])
        k.act(LN, k.banks[b][:, :], AF.Ln, [pk, "CSTF"], ["LN"], bias=k.cf("epsc", 1), scale=1.0 / D)
        k.ps_put(b)
        k.act(rs, LN, AF.Exp, ["LN"], [rsk], scale=-0.5)
        for c in range(8):
            k.stt(yt[:, c, :], k.XT[:, c, t0:t1], k.VEC[:, VO["gfin"] + c:VO["gfin"] + c + 1], rs, ALU.mult, ALU.mult,
                  ["XT", "VEC", rsk], [ytk])
        for tt_ in range(4):
            tok0 = (t0 - TC) + tt_ * 128
            ost, ostk = OST[n % 2], "OST%d" % (n % 2)
            n += 1
            for half in range(2):
                b = k.ps_get()
                pk = "ps%d" % b
                for j in range(4):
                    c = half * 4 + j
                    k.tr(k.banks[b][:, j * 128:(j + 1) * 128], yt[:, c, tt_ * 128:(tt_ + 1) * 128], k.cf("ident"),
                         [ytk, "CSTF"], [pk])
                k.cp(DVE if half == 0 else ACT, ost[:, half * 512:(half + 1) * 512], k.banks[b][:, :], [pk], [ostk])
                k.ps_put(b)
            k.dma(SP, dr["out"][tok0:tok0 + 128, :], ost, [ostk], [], "outd%d" % ((n - 1) % 2))
    P.barrier()


def _consts():
    cstb = np.zeros((128, NCB), np.float32)
    cstb[:, CB["ident"]:CB["ident"] + 128] = np.eye(128)
    cstb[:, CB["ones"]:CB["ones"] + 128] = 1.0
    bd = np.zeros((128, 128), np.float32)
    bd[:64, :64] = 1.0
    bd[64:, 64:] = 1.0
    cstb[:, CB["bd64"]:CB["bd64"] + 128] = bd
    rp = np.zeros((128, 128), np.float32)
    for r in range(128):
        p = (r % 32) // 16
        rp[r, r + 16 if p == 0 else r - 16] = 1.0
    cstb[:, CB["rperm"]:CB["rperm"] + 128] = rp
    s = np.arange(128)
    cstb[:, CB["tril"]:CB["tril"] + 128] = (s[:, None] <= s[None, :])
    cstb[:, CB["triu"]:CB["triu"] + 128] = (s[:, None] >= s[None, :])
    cstb[:, CB["iotaf"]:CB["iotaf"] + 256] = np.arange(256)[None, :]
    for e in range(16):
        cstb[e, CB["sel"] + e * 128:CB["sel"] + (e + 1) * 128] = 1.0
    cstf = np.zeros((128, NCF), np.float32)
    cstf[:, CF["ident"]:CF["ident"] + 128] = np.eye(128)
    cstf[:, CF["tril"]:CF["tril"] + 128] = (s[:, None] <= s[None, :])
    cstf[:, CF["triu"]:CF["triu"] + 128] = (s[:, None] >= s[None, :])
    cstf[:, CF["ones"]:CF["ones"] + 128] = 1.0
    cstf[:, CF["iotac"]] = np.arange(128)
    cstf[:, CF["iotac"] + 1] = np.arange(128) + 128
    cstf[:, CF["epsc"]] = EPS
    cstf[:, CF["onec"]] = 1.0
    rows = TL // 64
    row_ids = np.repeat(np.arange(rows, dtype=np.float32), 64)
    col_ids = np.tile(np.arange(64, dtype=np.float32), rows)
    half = 32
    inv = (np.float32(10000.0) ** (-np.arange(0, half, 2, dtype=np.float32) / np.float32(half))).astype(np.float32)
    rope = np.zeros((2, 128, TL), np.float32)
    for r in range(128):
        i = r % 64
        a, p, j = i // 32, (i % 32) // 16, i % 16
        ang = (row_ids if a == 0 else col_ids) * inv[j]
        rope[0, r] = np.cos(ang.astype(np.float32))
        rope[1, r] = np.sin(ang.astype(np.float32)) * (-1.0 if p == 0 else 1.0)
    invcnt = np.zeros((4, 128, NT), np.float32)
    for g, w in enumerate(POOL_W):
        lo = w // 2
        hi = w - 1 - lo
        for (s0, s1) in SEGS:
            n = s1 - s0
            t = np.arange(n)
            a = np.clip(t - lo, 0, n - 1)
            e = np.clip(t + hi, 0, n - 1)
            invcnt[g, :, s0:s1] = (1.0 / (e - a + 1).astype(np.float32))[None, :]
    return cstb, cstf, rope, invcnt


_CACHE = {}


def kernel(x, c, ctx, c_ctx, w_mod, b_mod, g_norm1, g_norm2, w_in_even, w_out_even, g_qnorm, g_knorm,
           w_conv, b_gate, g_hnorm, w_in_odd, w_pool_grp, s_pool, w_router, w_exp_gate, w_exp_up,
           w_exp_down, g_final, _dbg=False, _stop=None, _ncores=8):
    f = lambda a: np.ascontiguousarray(np.asarray(a, dtype=np.float32))
    x, c, ctx, c_ctx = f(x), f(c), f(ctx), f(c_ctx)
    cstb, cstf, rope, invcnt = _consts()
    vec_common = np.zeros((NV, 128), np.float32)

    def put(name, arr):
        a = f(arr).reshape(-1, 128)
        vec_common[VO[name]:VO[name] + a.shape[0]] = a
    put("bmod", b_mod)
    put("gn1", g_norm1)
    put("gn2", g_norm2)
    put("gfin", g_final)
    put("spool", s_pool)
    put("wconv", w_conv)
    put("ghn", g_hnorm)
    put("gq", np.tile(f(g_qnorm), (1, 2)))
    put("gk", np.tile(f(g_knorm), (1, 2)))
    put("cctx", c_ctx)
    bgate = np.ascontiguousarray(np.tile(f(b_gate).reshape(1, 32), (128, 1)))
    shared = {"cstb": cstb, "cstf": cstf, "rope": rope, "bgate": bgate, "invcnt": invcnt,
              "w_mod": f(w_mod), "w_in_even": f(w_in_even), "w_out_even": f(w_out_even), "w_in_odd": f(w_in_odd),
              "w_pool_grp": f(w_pool_grp), "w_router": f(w_router), "w_exp_gate": f(w_exp_gate),
              "w_exp_up": f(w_exp_up), "w_exp_down": f(w_exp_down)}
    in_maps = []
    for b in range(_ncores):
        v = vec_common.copy()
        v[VO["cvec"]:VO["cvec"] + 8] = c[b].reshape(8, 128)
        m = dict(shared)
        m["xin"] = np.ascontiguousarray(np.concatenate([ctx[b], x[b]], axis=0))
        m["vecs"] = v
        in_maps.append(m)
    key = (_dbg, _stop)
    if key not in _CACHE:
        _CACHE[key] = build(dbg=_dbg, stop_after=_stop)
    nc = _CACHE[key]
    res = run_bass_kernel_spmd(nc, in_maps, core_ids=list(range(_ncores)))
    out = np.stack([np.asarray(r["out"], dtype=np.float32) for r in res.results], axis=0)
    if _dbg:
        return out, [np.asarray(r["dbg"]) for r in res.results]
    return out


def _proj_fm(k, Wb, wk, HT, t0, t1):
    b = k.ps_get()
    for kk in range(8):
        k.mm(k.banks[b][:, 0:t1 - t0], Wb[:, kk, :], HT[:, kk, t0:t1], kk == 0, kk == 7, [wk, "HT"], ["ps%d" % b])
    return b


def phase_even(k, i):
    P, dr, ar = k.P, k.dr, k.ar
    ev = i // 2
    ctx_out = CTX_NEXT[i]
    W_in = dr["w_in_even"][ev]
    ar.reset()
    HT = ar.alloc([8, NT], BF16)
    mark_ht = ar.off
    norm_mod(k, i, 0, HT, include_ctx=True)
    P.barrier()
    ar.reset(mark_ht)
    wcol = lambda c0, c1: W_in[:, c0:c1].rearrange("(c p) n -> p c n", p=128)
    oblocks = BLOCKS if ctx_out else BLOCKS[1:]

    def out_proj(CATx, half):
        WO = ar.alloc([4, 1024], BF16)
        k.wload(WO, dr["w_out_even"][ev][half * 512:(half + 1) * 512, :].rearrange("(c p) n -> p c n", p=128), "WO")
        for dmc in range(8):
            for (t0, t1) in oblocks:
                tw = t1 - t0
                w = 1 if t0 < TC else 0
                b = k.ps_get()
                pk = "ps%d" % b
                for f in range(4):
                    k.mm(k.banks[b][:, 0:tw], WO[:, f, dmc * 128:(dmc + 1) * 128], CATx[:, f, t0:t1], f == 0, f == 3,
                         ["WO", "CAT"], [pk])
                xs = k.XT[:, dmc, t0:t1]
                k.stt(xs, k.banks[b][:, 0:tw], k.MODX[:, i, w, 2, dmc:dmc + 1], xs, ALU.mult, ALU.add,
                      [pk, "MODX", "XT"], ["XT"])
                k.ps_put(b)

    CAT = ar.alloc([4, NT], BF16)
    KT = [ar.alloc([NT], BF16) for _ in range(2)]
    VA = ar.alloc([18, 2, 129], BF16)
    QTb = ar.alloc([4, 512], BF16)
    COS = ar.alloc([512], F32)
    SIN = ar.alloc([512], F32)
    WB = [ar.alloc([8, 128], BF16) for _ in range(3)]
    SQ = ar.alloc([512], BF16)
    LNs = ar.alloc([512], F32)
    RS = ar.alloc([512], F32)
    QN = ar.alloc([512], F32)
    QNB = ar.alloc([512], BF16)
    T1 = ar.alloc([512], F32)
    T2 = ar.alloc([512], F32)
    PTT = [ar.alloc([512], BF16) for _ in range(3)]
    DROW = ar.alloc([512], F32)
    LND = ar.alloc([512], F32)
    RD = ar.alloc([512], F32)
    wbn = [0]

    def wb_load(parts):
        s = wbn[0] % 3
        wbn[0] += 1
        o = 0
        for (c0, c1) in parts:
            k.wload(WB[s][:, :, o:o + (c1 - c0)], wcol(c0, c1), "WB%d" % s)
            o += c1 - c0
        return WB[s], "WB%d" % s

    def qk_norm_rope(b, tw, t0, gname, dest, rope_ok):
        pk = "ps%d" % b
        k.act(SQ[:, 0:tw], k.banks[b][:, 0:tw], AF.Square, [pk], ["SQ"])
        b2 = k.ps_get()
        k.mm(k.banks[b2][:, 0:tw], k.cb("bd64"), SQ[:, 0:tw], True, True, ["SQ", "CSTB"], ["ps%d" % b2])
        k.act(LNs[:, 0:tw], k.banks[b2][:, 0:tw], AF.Ln, ["ps%d" % b2, "CSTF"], ["LNs"], bias=k.cf("epsc", 1),
              scale=1.0 / 64)
        k.ps_put(b2)
        k.act(RS[:, 0:tw], LNs[:, 0:tw], AF.Exp, ["LNs"], ["RS"], scale=-0.5)
        gcol = k.VEC[:, VO[gname] + ev:VO[gname] + ev + 1]
        if not rope_ok:
            k.stt(dest, k.banks[b][:, 0:tw], gcol, RS[:, 0:tw], ALU.mult, ALU.mult, [pk, "VEC", "RS"], ["QK"])
            return
        k.stt(QN[:, 0:tw], k.banks[b][:, 0:tw], gcol, RS[:, 0:tw], ALU.mult, ALU.mult, [pk, "VEC", "RS"], ["QN"])
        k.cp(ACT, QNB[:, 0:tw], QN[:, 0:tw], ["QN"], ["QNB"])
        b3 = k.ps_get()
        k.mm(k.banks[b3][:, 0:tw], k.cb("rperm"), QNB[:, 0:tw], True, True, ["QNB", "CSTB"], ["ps%d" % b3])
        k.tt(POOL, T1[:, 0:tw], QN[:, 0:tw], COS[:, 0:tw], ALU.mult, ["QN", "COS"], ["T1"])
        k.tt(DVE, T2[:, 0:tw], k.banks[b3][:, 0:tw], SIN[:, 0:tw], ALU.mult, ["ps%d" % b3, "SIN"], ["T2"])
        k.ps_put(b3)
        k.tt(DVE, dest, T1[:, 0:tw], T2[:, 0:tw], ALU.add, ["T1", "T2"], ["QK"])

    def load_rope(t0):
        k.dma(SP, COS, dr["rope"][0][:, t0 - TC:t0 - TC + 512], [], ["COS"], "COS")
        k.dma(SP, SIN, dr["rope"][1][:, t0 - TC:t0 - TC + 512], [], ["SIN"], "SIN")

    k.P.op(POOL, lambda e: e.memset(VA[:, :, :, :], 0.0), writes=["VA"])
    k.P.op(POOL, lambda e: e.memset(VA[:, :, :, 0:1], 1.0), writes=["VA"])
    k.P.op(POOL, lambda e: e.memset(VA[:, :, :, 128:129], 1.0), writes=["VA"])
    Wv, wvk = wb_load([(640, 768)])
    for t in range(18):
        b = k.ps_get()
        for kk in range(8):
            k.mm(k.banks[b][:, 0:128], HT[:, kk, t * 128:(t + 1) * 128], Wv[:, kk, :], kk == 0, kk == 7, ["HT", wvk],
                 ["ps%d" % b])
        k.cp(ACT if t % 2 else DVE, VA[:, t, :, 64:128], k.banks[b][:, 0:128].rearrange("p (a b) -> p a b", a=2),
             ["ps%d" % b], ["VA"])
        k.ps_put(b)
    for v, parts in enumerate(([(512, 640)], [(576, 640), (512, 576)])):
        Wk, wkk = wb_load(parts)
        for (t0, t1) in BLOCKS:
            if t0 >= TC:
                load_rope(t0)
            b = _proj_fm(k, Wk, wkk, HT, t0, t1)
            qk_norm_rope(b, t1 - t0, t0, "gk", KT[v][:, t0:t1], t0 >= TC)
            k.ps_put(b)
    for (t0, t1) in oblocks:
        tw = t1 - t0
        is_lat = t0 >= TC
        if is_lat:
            load_rope(t0)
        for qc in range(4):
            Wq, wqk = wb_load([(qc * 128, (qc + 1) * 128)])
            b = _proj_fm(k, Wq, wqk, HT, t0, t1)
            qk_norm_rope(b, tw, t0, "gq", QTb[:, qc, 0:tw], is_lat)
            k.ps_put(b)
        stiles = list(range(18)) if is_lat else [0, 1]
        for h in range(8):
            kv, hf, qc = h // 4, h % 2, h // 2
            KTh = KT[0] if kv == hf else KT[1]
            r0 = hf * 64
            bo = k.ps_get()
            pko = "ps%d" % bo
            M = 65 if hf == 0 else 128
            nst = len(stiles)
            bsl = {}

            def issue_S(si):
                s = stiles[si]
                bs = k.ps_get()
                k.mm(k.banks[bs][:, 0:tw], KTh[r0:r0 + 64, s * 128:(s + 1) * 128], QTb[r0:r0 + 64, qc, 0:tw], True, True,
                     ["QK"], ["ps%d" % bs])
                bsl[si] = bs
            for si in range(min(2, nst)):
                issue_S(si)
            for si, s in enumerate(stiles):
                bs = bsl.pop(si)
                pt = PTT[si % 3]
                ptk = "PTT%d" % (si % 3)
                k.act(pt[:, 0:tw], k.banks[bs][:, 0:tw], AF.Exp, ["ps%d" % bs], [ptk], scale=0.125)
                k.ps_put(bs)
                if si + 2 < nst:
                    issue_S(si + 2)
                lv = VA[:, s, kv, 64:129] if hf == 0 else VA[:, s, kv, 0:128]
                k.mm(k.banks[bo][0:M, 0:tw], lv, pt[:, 0:tw], si == 0, si == nst - 1, ["VA", ptk], [pko])
            drow = 64 if hf == 0 else 0
            k.cp(ACT, DROW[drow:drow + 1, 0:tw], k.banks[bo][drow:drow + 1, 0:tw], [pko], ["DROW"])
            bd = k.ps_get()
            nrow = 64 if hf == 0 else 128
            k.mm(k.banks[bd][0:nrow, 0:tw], k.CSTF[drow:drow + 1, CF["ones"]:CF["ones"] + nrow], DROW[drow:drow + 1, 0:tw],
                 True, True, ["DROW", "CSTF"], ["ps%d" % bd])
            k.act(LND[r0:r0 + 64, 0:tw], k.banks[bd][r0:r0 + 64, 0:tw], AF.Ln, ["ps%d" % bd], ["LND"])
            k.ps_put(bd)
            k.act(RD[r0:r0 + 64, 0:tw], LND[r0:r0 + 64, 0:tw], AF.Exp, ["LND"], ["RD"], scale=-1.0)
            k.tt(DVE, CAT[r0:r0 + 64, qc, t0:t1], k.banks[bo][r0:r0 + 64, 0:tw], RD[r0:r0 + 64, 0:tw], ALU.mult,
                 [pko, "RD"], ["CAT"])
            k.ps_put(bo)
    out_proj(CAT, 0)
    P.barrier()
    ar.reset(mark_ht)

    CAT = ar.alloc([4, NT], BF16)
    mark_cat = ar.off
    GT = ar.alloc([18, 16], F32)
    NLF = ar.alloc([18, 8], F32)
    IG = ar.alloc([18, 8], F32)
    NB = ar.alloc([18, 8], F32)
    NTOT = ar.alloc([18, 8], F32)
    EXA = ar.alloc([18, 8], F32)
    US = ar.alloc([18, 8], F32)
    WSC = ar.alloc([18, 8], F32)
    DEC = ar.alloc([18, 8], F32)
    WB = [ar.alloc([8, 128], BF16) for _ in range(3)]
    VB = ar.alloc([18, 129], BF16)
    PADQ = 2
    XQW = PADQ + TC + PADQ + PADQ + TL + PADQ
    qoff = [PADQ, PADQ + TC + PADQ + PADQ]
    XQ = ar.alloc([XQW], F32)
    ACC = ar.alloc([NT], F32)
    QM = ar.alloc([NT], BF16)
    KM = ar.alloc([NT], BF16)
    KW = [[ar.alloc([128], BF16) for _ in range(2)] for _ in range(2)]
    HDb = ar.alloc([NT], BF16)
    HM = XQ
    CT = [ar.alloc([129], F32) for _ in range(2)]
    CTB = [[ar.alloc([128], BF16) for _ in range(2)] for _ in range(2)]
    NREP = [[ar.alloc([128], BF16) for _ in range(2)] for _ in range(2)]
    NLFR = [ar.alloc([128], F32) for _ in range(2)]
    SMU = [[ar.alloc([128], BF16) for _ in range(2)] for _ in range(2)]
    EB = [[ar.alloc([128], F32) for _ in range(2)] for _ in range(2)]
    DEN = [ar.alloc([128], F32) for _ in range(2)]
    RDM = [ar.alloc([128], F32) for _ in range(2)]
    SQm = ar.alloc([512], BF16)
    SGm = ar.alloc([512], BF16)
    TMm = ar.alloc([512], F32)
    scale = 128.0 ** -0.5
    Wg_ = ar.alloc([8, 16], BF16)
    k.wload(Wg_, wcol(2816, 2832), "Wg_")
    b = k.ps_get()
    for t in range(18):
        for kk in range(8):
            k.mm(k.banks[b][:, t * 16:(t + 1) * 16], HT[:, kk, t * 128:(t + 1) * 128], Wg_[:, kk, :], kk == 0, kk == 7,
                 ["HT", "Wg_"], ["ps%d" % b])
    k.tt(DVE, GT, k.banks[b][:, 0:288].rearrange("p (a b) -> p a b", b=16),
         k.BG[:, ev * 16:(ev + 1) * 16].unsqueeze(1).to_broadcast([128, 18, 16]), ALU.add, ["ps%d" % b, "BG"], ["GT"])
    k.ps_put(b)
    for d in range(2):
        k.cp(DVE, IG[:, :, d * 4:(d + 1) * 4], GT[:, :, d * 8:d * 8 + 4], ["GT"], ["IG"])
        k.act(NLF[:, :, d * 4:(d + 1) * 4], GT[:, :, d * 8 + 4:d * 8 + 8], AF.Exp, ["GT"], ["NLF"], scale=-1.0)
    k.act(NLF, NLF, AF.Ln, ["NLF", "CSTF"], ["NLF"], bias=k.cf("onec", 1), scale=1.0)
    b = k.ps_get()
    for d in range(2):
        for j in range(18):
            k.mm(k.banks[b][:, j * 8 + d * 4:j * 8 + d * 4 + 4], k.cf("tril" if d == 0 else "triu"),
                 NLF[:, j, d * 4:(d + 1) * 4], True, True, ["NLF", "CSTF"], ["ps%d" % b])
    k.cp(DVE, NB, k.banks[b][:, 0:144].rearrange("p (a b) -> p a b", b=8), ["ps%d" % b], ["NB"])
    k.ps_put(b)
    b = k.ps_get()
    k.mm(k.banks[b][:, 0:144], k.cf("ones"), NLF.rearrange("p a b -> p (a b)"), True, True, ["NLF", "CSTF"], ["ps%d" % b])
    k.cp(DVE, NTOT, k.banks[b][:, 0:144].rearrange("p (a b) -> p a b", b=8), ["ps%d" % b], ["NTOT"])
    k.ps_put(b)
    k.tt(DVE, EXA, IG, NB, ALU.add, ["IG", "NB"], ["EXA"])
    k.act(US, EXA, AF.Exp, ["EXA"], ["US"])
    k.ts(DVE, US, US, scale, None, ALU.mult, None, ["US"], ["US"])
    k.tt(DVE, EXA, EXA, NTOT, ALU.subtract, ["EXA", "NTOT"], ["EXA"])
    k.act(WSC, EXA, AF.Exp, ["EXA"], ["WSC"])
    k.ts(DVE, WSC, WSC, scale, None, ALU.mult, None, ["WSC"], ["WSC"])
    k.act(DEC, NTOT, AF.Exp, ["NTOT"], ["DEC"], scale=-1.0)
    orders = [list(range(18)), [1, 0] + list(range(17, 1, -1))]
    k.P.op(POOL, lambda e: e.memset(XQ[:, :], 0.0), writes=["XQ"])
    for hd in range(4):
        HD = [CAT[:, hd, :], HDb]
        k.P.op(POOL, lambda e: e.memset(VB[:, :, 128:129], 1.0), writes=["VB"])
        Wv, wvk = None, None
        s = wbn[0] % 3
        wbn[0] += 1
        k.wload(WB[s], wcol(1792 + hd * 128, 1792 + (hd + 1) * 128), "WB%d" % s)
        Wv, wvk = WB[s], "WB%d" % s
        for t in range(18):
            b = k.ps_get()
            for kk in range(8):
                k.mm(k.banks[b][:, 0:128], HT[:, kk, t * 128:(t + 1) * 128], Wv[:, kk, :], kk == 0, kk == 7, ["HT", wvk],
                     ["ps%d" % b])
            k.cp(ACT if t % 2 else DVE, VB[:, t, 0:128], k.banks[b][:, 0:128], ["ps%d" % b], ["VB"])
            k.ps_put(b)
        for which, (c0, dest) in enumerate(((768 + hd * 128, QM), (1280 + hd * 128, KM))):
            s = wbn[0] % 3
            wbn[0] += 1
            k.wload(WB[s], wcol(c0, c0 + 128), "WB%d" % s)
            for (t0, t1) in BLOCKS:
                b = _proj_fm(k, WB[s], "WB%d" % s, HT, t0, t1)
                si = 0 if t0 < TC else 1
                off = qoff[si] + (t0 - SEGS[si][0])
                k.cp(ACT, XQ[:, off:off + (t1 - t0)], k.banks[b][:, 0:t1 - t0], ["ps%d" % b], ["XQ"])
                k.ps_put(b)
            cch = which * 4 + hd
            wc = lambda j: k.VEC[:, VO["wconv"] + (ev * 3 + j) * 8 + cch:VO["wconv"] + (ev * 3 + j) * 8 + cch + 1]
            for si, (s0, s1) in enumerate(SEGS):
                n = s1 - s0
                o = qoff[si]
                k.ts(DVE, ACC[:, s0:s1], XQ[:, o:o + n], wc(1), None, ALU.mult, None, ["XQ", "VEC"], ["ACC"])
                k.stt(ACC[:, s0:s1], XQ[:, o - 1:o - 1 + n], wc(0), ACC[:, s0:s1], ALU.mult, ALU.add, ["XQ", "VEC", "ACC"],
                      ["ACC"])
                k.stt(ACC[:, s0:s1], XQ[:, o + 1:o + 1 + n], wc(2), ACC[:, s0:s1], ALU.mult, ALU.add, ["XQ", "VEC", "ACC"],
                      ["ACC"])
            k.act(dest, ACC, AF.Silu, ["ACC"], ["QM" if which == 0 else "KM"])
        for d in range(2):
            k.P.op(POOL, lambda e, d=d: e.memset(CT[d][:, :], 0.0), writes=["CT%d" % d])
            k.P.op(POOL, lambda e, d=d: e.memset(CTB[d][0][:, :], 0.0), writes=["CTB%d0" % d])
            k.P.op(POOL, lambda e, d=d: e.memset(NREP[d][0][:, :], 0.0), writes=["NREP%d0" % d])
        bcl = {}

        def indep(step):
            for d in range(2):
                j = orders[d][step]
                gi = d * 4 + hd
                cols = slice(j * 128, (j + 1) * 128)
                pp = step % 2
                msk = k.cb("tril" if d == 0 else "triu")
                bs = k.ps_get()
                k.mm(k.banks[bs][:, 0:128], KM[:, cols], QM[:, cols], True, True, ["KM", "QM"], ["ps%d" % bs])
                k.stt(SMU[d][pp], k.banks[bs][:, 0:128], US[:, j, gi:gi + 1], msk, ALU.mult, ALU.mult,
                      ["ps%d" % bs, "US", "CSTB"], ["SMU%d%d" % (d, pp)])
                k.ps_put(bs)
                k.act(NLFR[d], k.cf("ones"), AF.Identity, ["CSTF", "NLF"], ["NLFR%d" % d], scale=NLF[:, j, gi:gi + 1])
                be = k.ps_get()
                k.mm(k.banks[be][:, 0:128], NLFR[d], k.cf("tril" if d == 0 else "triu"), True, True,
                     ["NLFR%d" % d, "CSTF"], ["ps%d" % be])
                k.act(EB[d][pp], k.banks[be][:, 0:128], AF.Exp, ["ps%d" % be], ["EB%d%d" % (d, pp)])
                k.ps_put(be)
                if step < 17:
                    bt = k.ps_get()
                    pv = k.banks[bt][:, 0:64].bitcast(BF16)
                    k.tr(pv, KM[:, cols], k.cb("ident"), ["KM", "CSTB"], ["ps%d" % bt])
                    k.act(KW[d][pp], pv, AF.Identity, ["ps%d" % bt, "WSC"], ["KW%d%d" % (d, pp)], scale=WSC[:, j, gi:gi + 1])
                    k.ps_put(bt)
                    bc = k.ps_get()
                    k.mm(k.banks[bc][:, 0:129], KW[d][pp], VB[:, j, :], True, True, ["KW%d%d" % (d, pp), "VB"], ["ps%d" % bc])
                    bcl[(step, d)] = bc

        def dep(step):
            pp = step % 2
            pn = (step + 1) % 2
            held = []
            for d in range(2):
                j = orders[d][step]
                gi = d * 4 + hd
                cols = slice(j * 128, (j + 1) * 128)
                bx = k.ps_get()
                k.mm(k.banks[bx][:, 0:128], VB[:, j, 0:128], SMU[d][pp], True, False, ["VB", "SMU%d%d" % (d, pp)], ["ps%d" % bx])
                k.mm(k.banks[bx][:, 0:128], CTB[d][pp], QM[:, cols], False, True, ["CTB%d%d" % (d, pp), "QM"], ["ps%d" % bx])
                by = k.ps_get()
                k.mm(k.banks[by][:, 0:128], k.cb("ones"), SMU[d][pp], True, False, ["CSTB", "SMU%d%d" % (d, pp)], ["ps%d" % by])
                k.mm(k.banks[by][:, 0:128], NREP[d][pp], QM[:, cols], False, True, ["NREP%d%d" % (d, pp), "QM"], ["ps%d" % by])
                held.append((d, j, cols, bx, by))
            if step < 17:
                for d in range(2):
                    j = orders[d][step]
                    gi = d * 4 + hd
                    bc = bcl.pop((step, d))
                    k.stt(CT[d], CT[d], DEC[:, j, gi:gi + 1], k.banks[bc][:, 0:129], ALU.mult, ALU.add,
                          ["CT%d" % d, "DEC", "ps%d" % bc], ["CT%d" % d])
                    k.ps_put(bc)
                    k.cp(ACT, CTB[d][pn], CT[d][:, 0:128], ["CT%d" % d], ["CTB%d%d" % (d, pn)])
                    k.act(NREP[d][pn], k.cb("ones"), AF.Identity, ["CSTB", "CT%d" % d], ["NREP%d%d" % (d, pn)],
                          scale=CT[d][:, 128:129])
            for (d, j, cols, bx, by) in held:
                k.tt(DVE, DEN[d], k.banks[by][:, 0:128], EB[d][pp], ALU.max, ["ps%d" % by, "EB%d%d" % (d, pp)], ["DEN%d" % d])
                k.stt(DEN[d], k.banks[by][:, 0:128], -1.0, DEN[d], ALU.mult, ALU.max, ["ps%d" % by, "DEN%d" % d],
                      ["DEN%d" % d])
                k.ps_put(by)
                k.act(RDM[d], DEN[d], AF.Ln, ["DEN%d" % d], ["RDM%d" % d])
                k.act(RDM[d], RDM[d], AF.Exp, ["RDM%d" % d], ["RDM%d" % d], scale=-1.0)
                k.tt(DVE, HD[d][:, cols], k.banks[bx][:, 0:128], RDM[d], ALU.mult, ["ps%d" % bx, "RDM%d" % d], ["HD%d" % d])
                k.ps_put(bx)

        indep(0)
        for step in range(18):
            if step + 1 < 18:
                indep(step + 1)
            dep(step)
        HMv = HM[:, 0:NT]
        k.tt(POOL, HMv, HD[0], HD[1], ALU.add, ["HD0", "HD1"], ["XQ"])
        s = wbn[0] % 3
        wbn[0] += 1
        k.wload(WB[s], wcol(2304 + hd * 128, 2304 + (hd + 1) * 128), "WB%d" % s)
        for (t0, t1) in oblocks:
            tw = t1 - t0
            k.act(SQm[:, 0:tw], HMv[:, t0:t1], AF.Square, ["XQ"], ["SQm"])
            b2 = k.ps_get()
            k.mm(k.banks[b2][:, 0:tw], k.cb("ones"), SQm[:, 0:tw], True, True, ["SQm", "CSTB"], ["ps%d" % b2])
            k.act(TMm[:, 0:tw], k.banks[b2][:, 0:tw], AF.Ln, ["ps%d" % b2, "CSTF"], ["TMm"], bias=k.cf("epsc", 1),
                  scale=1.0 / 128)
            k.ps_put(b2)
            k.act(TMm[:, 0:tw], TMm[:, 0:tw], AF.Exp, ["TMm"], ["TMm"], scale=-0.5)
            k.stt(TMm[:, 0:tw], HMv[:, t0:t1], k.VEC[:, VO["ghn"] + ev * 4 + hd:VO["ghn"] + ev * 4 + hd + 1], TMm[:, 0:tw],
                  ALU.mult, ALU.mult, ["XQ", "VEC", "TMm"], ["TMm"])
            b = _proj_fm(k, WB[s], "WB%d" % s, HT, t0, t1)
            k.act(SGm[:, 0:tw], k.banks[b][:, 0:tw], AF.Sigmoid, ["ps%d" % b], ["SGm"])
            k.ps_put(b)
            k.tt(DVE, CAT[:, hd, t0:t1], TMm[:, 0:tw], SGm[:, 0:tw], ALU.mult, ["TMm", "SGm"], ["CAT"])
        if hd < 3:
            k.P.op(POOL, lambda e: e.memset(XQ[:, :], 0.0), writes=["XQ"])
    P.barrier()
    ar.reset(mark_cat)
    out_proj(CAT, 1)
    P.barrier()
```
